# Optimizing a Trainium2 kernel written in Bass

```python
import math
import jax
import jax.numpy as jnp
from jax import lax
import numpy as np

D_MODEL = 1024
BATCH = 4
SEQ = 8192
DEPTH = 1

F32 = jnp.float32
HEAD_DIM = 128
GDN_HEADS = 4
GDN_WIDTH = GDN_HEADS * HEAD_DIM
ATTN_Q_HEADS = 4
ATTN_KV_HEADS = 2
ATTN_WIDTH = ATTN_Q_HEADS * HEAD_DIM
ATTN_KV_WIDTH = ATTN_KV_HEADS * HEAD_DIM
MIX_WIDTH = GDN_WIDTH + ATTN_WIDTH
IN_SPLITS = (GDN_WIDTH, GDN_WIDTH, GDN_WIDTH, GDN_WIDTH, 4 * GDN_HEADS, ATTN_WIDTH, ATTN_KV_WIDTH, ATTN_KV_WIDTH)
IN_COLS = sum(IN_SPLITS)
CONV_K = 5
CHUNK = 64
Q_BLOCK = 128
GRID_W = 64
ROPE_THETA = 10000.0
N_GROUPS = 4
EXPERTS_PER_GROUP = 8
N_EXPERTS = N_GROUPS * EXPERTS_PER_GROUP
TOP_K_IN_GROUP = 2
D_EXPERT = 256
MOE_BLOCK = 128
ALPHA = (2.0 * DEPTH) ** 0.25
BETA = (8.0 * DEPTH) ** -0.25
LN_EPS = 1e-5
RMS_EPS = 1e-6

kernel_name = 'hybrid_gdn_axialgqa_hmoe_block'


def layer_norm(x, gain=None, bias=None):
    xf = x.astype(F32)
    mu = jnp.mean(xf, axis=-1, keepdims=True)
    var = jnp.mean(jnp.square(xf - mu), axis=-1, keepdims=True)
    y = (xf - mu) * lax.rsqrt(var + LN_EPS)
    if gain is not None:
        y = y * gain.astype(F32) + bias.astype(F32)
    return y.astype(x.dtype)


def rms_norm(x, gain):
    xf = x.astype(F32)
    y = xf * lax.rsqrt(jnp.mean(jnp.square(xf), axis=-1, keepdims=True) + RMS_EPS)
    return (y * gain.astype(F32)).astype(x.dtype)


def l2_normalize(x):
    xf = x.astype(F32)
    return (xf * lax.rsqrt(jnp.sum(jnp.square(xf), axis=-1, keepdims=True) + RMS_EPS)).astype(x.dtype)


def centred_short_conv(x, w):
    pad = CONV_K // 2
    y = lax.conv_general_dilated(x, w[:, None, :].astype(x.dtype), window_strides=(1,), padding=[(pad, pad)],
                                 dimension_numbers=('NWC', 'WIO', 'NWC'), feature_group_count=x.shape[-1])
    return jax.nn.silu(y)


def chunk_gated_delta_rule(q, k, v, g, beta):
    b, h, s, dk = q.shape
    dv = v.shape[-1]
    n = s // CHUNK
    out_dtype = v.dtype
    q, k, v = [t.astype(F32).reshape(b, h, n, CHUNK, t.shape[-1]) for t in (q, k, v)]
    g = jnp.cumsum(g.astype(F32).reshape(b, h, n, CHUNK), axis=-1)
    beta = beta.astype(F32).reshape(b, h, n, CHUNK)
    idx = jnp.arange(CHUNK)
    lower_incl = idx[:, None] >= idx[None, :]
    strict = idx[:, None] > idx[None, :]
    decay = jnp.exp(jnp.where(lower_incl, g[..., :, None] - g[..., None, :], -jnp.inf))
    kk = jnp.einsum('bhncd,bhnsd->bhncs', k, k)
    m = jnp.where(strict, beta[..., :, None] * kk * decay, 0.0)
    eye = jnp.eye(CHUNK, dtype=F32)
    rhs = jnp.concatenate([v * beta[..., None], k * (beta * jnp.exp(g))[..., None]], axis=-1)
    sol = lax.linalg.triangular_solve(eye + m, rhs, left_side=True, lower=True, unit_diagonal=True)
    u, w = sol[..., :dv], sol[..., dv:]
    qk = jnp.einsum('bhncd,bhnsd->bhncs', q, k) * decay

    def step(state, xs):
        q_c, k_c, u_c, w_c, g_c, qk_c = xs
        v_new = u_c - jnp.einsum('bhck,bhkv->bhcv', w_c, state)
        o = jnp.einsum('bhck,bhkv->bhcv', q_c * jnp.exp(g_c)[..., None], state) + jnp.einsum('bhcs,bhsv->bhcv', qk_c, v_new)
        g_last = g_c[..., -1]
        state = state * jnp.exp(g_last)[..., None, None] + jnp.einsum(
            'bhck,bhcv->bhkv', k_c * jnp.exp(g_last[..., None] - g_c)[..., None], v_new)
        return state, o

    xs = tuple(jnp.moveaxis(t, 2, 0) for t in (q, k, u, w, g, qk))
    _, o = lax.scan(step, jnp.zeros((b, h, dk, dv), F32), xs)
    return jnp.moveaxis(o, 0, 2).reshape(b, h, s, dv).astype(out_dtype)


def gdn_mixer(q, k, v, z, ab, conv_w, a_log, dt_bias, norm_g):
    b, s, _ = q.shape
    qkv = centred_short_conv(jnp.concatenate([q, k, v], axis=-1), conv_w)
    q, k, v = jnp.split(qkv, 3, axis=-1)
    to_heads = lambda t: t.reshape(b, s, GDN_HEADS, HEAD_DIM).transpose(0, 2, 1, 3)
    q = l2_normalize(to_heads(q)) * (HEAD_DIM ** -0.5)
    k = l2_normalize(to_heads(k))
    v = to_heads(v)
    a_f, a_b, b_f, b_b = jnp.split(ab.astype(F32), 4, axis=-1)

    def decay_and_beta(a, bb, d):
        g = -jnp.exp(a_log[d].astype(F32)) * jax.nn.softplus(a + dt_bias[d].astype(F32))
        return g.transpose(0, 2, 1), jax.nn.sigmoid(bb).transpose(0, 2, 1)

    g_f, beta_f = decay_and_beta(a_f, b_f, 0)
    g_b, beta_b = decay_and_beta(a_b, b_b, 1)
    rev = lambda t: jnp.flip(t, axis=2)
    o_f = chunk_gated_delta_rule(q, k, v, g_f, beta_f)
    o_b = rev(chunk_gated_delta_rule(rev(q), rev(k), rev(v), rev(g_b), rev(beta_b)))
    o = (o_f + o_b).transpose(0, 2, 1, 3)
    o = rms_norm(o, norm_g) * jax.nn.silu(z.reshape(b, s, GDN_HEADS, HEAD_DIM))
    return o.reshape(b, s, GDN_WIDTH)


def rope_1d(x, pos):
    d = x.shape[-1]
    inv = ROPE_THETA ** (-jnp.arange(0, d, 2, dtype=F32) / d)
    ang = pos.astype(F32)[:, None] * inv[None, :]
    cos = jnp.cos(ang)[None, :, None, :]
    sin = jnp.sin(ang)[None, :, None, :]
    xf = x.astype(F32)
    x1, x2 = xf[..., : d // 2], xf[..., d // 2:]
    return jnp.concatenate([x1 * cos - x2 * sin, x2 * cos + x1 * sin], axis=-1).astype(x.dtype)


def axial_rope(x, row, col):
    half = x.shape[-1] // 2
    return jnp.concatenate([rope_1d(x[..., :half], row), rope_1d(x[..., half:], col)], axis=-1)


def blocked_gqa(q, k, v):
    b, s = q.shape[:2]
    n_blk = s // Q_BLOCK
    grp = ATTN_Q_HEADS // ATTN_KV_HEADS
    qb = q.reshape(b, n_blk, Q_BLOCK, ATTN_KV_HEADS, grp, HEAD_DIM).transpose(1, 0, 3, 4, 2, 5)
    kt = k.transpose(0, 2, 1, 3)
    vt = v.transpose(0, 2, 1, 3)
    scale = HEAD_DIM ** -0.5

    def one_block(q_blk):
        logits = jnp.einsum('bkgqd,bksd->bkgqs', q_blk, kt).astype(F32) * scale
        p = jax.nn.softmax(logits, axis=-1).astype(vt.dtype)
        return jnp.einsum('bkgqs,bksd->bkgqd', p, vt)

    o = lax.map(one_block, qb)
    return o.transpose(1, 0, 4, 2, 3, 5).reshape(b, s, ATTN_WIDTH)


def attention_mixer(q, k, v, q_norm_g, k_norm_g, out_norm_g):
    b, s, _ = q.shape
    q = rms_norm(q.reshape(b, s, ATTN_Q_HEADS, HEAD_DIM), q_norm_g)
    k = rms_norm(k.reshape(b, s, ATTN_KV_HEADS, HEAD_DIM), k_norm_g)
    v = v.reshape(b, s, ATTN_KV_HEADS, HEAD_DIM)
    n_rows = s // GRID_W
    row = jnp.repeat(jnp.arange(n_rows), GRID_W)
    col = jnp.tile(jnp.arange(GRID_W), n_rows)
    q = axial_rope(q, row, col)
    k = axial_rope(k, row, col)
    return rms_norm(blocked_gqa(q, k, v), out_norm_g)


def hierarchical_moe(h, w_group, b_group, w_router, b_router, w1, w3, w2):
    b, s, d = h.shape
    t = h.reshape(-1, d)
    n_tok = t.shape[0]
    grp_logits = (t @ w_group).astype(F32) + b_group.astype(F32)
    grp_prob = jax.nn.softmax(grp_logits, axis=-1)
    grp_idx = jnp.argmax(grp_logits, axis=-1)
    grp_p = jnp.take_along_axis(grp_prob, grp_idx[:, None], axis=-1)[:, 0]
    exp_logits = ((t @ w_router).astype(F32) + b_router.astype(F32)).reshape(n_tok, N_GROUPS, EXPERTS_PER_GROUP)
    in_grp = jnp.take_along_axis(exp_logits, grp_idx[:, None, None], axis=1)[:, 0]
    top_val, top_local = lax.top_k(in_grp, TOP_K_IN_GROUP)
    weights = grp_p[:, None] * jax.nn.softmax(top_val, axis=-1)
    expert_idx = grp_idx[:, None] * EXPERTS_PER_GROUP + top_local

    n_assign = n_tok * TOP_K_IN_GROUP
    flat_e = expert_idx.reshape(-1)
    flat_w = weights.reshape(-1)
    flat_tok = jnp.repeat(jnp.arange(n_tok), TOP_K_IN_GROUP)
    order = jnp.argsort(flat_e)
    e_sorted = flat_e[order]
    counts = jnp.bincount(flat_e, length=N_EXPERTS)
    padded = (counts + MOE_BLOCK - 1) // MOE_BLOCK * MOE_BLOCK
    pad_end = jnp.cumsum(padded)
    pad_start = pad_end - padded
    start = jnp.cumsum(counts) - counts
    dest = pad_start[e_sorted] + jnp.arange(n_assign) - start[e_sorted]
    n_blocks = -(-n_assign // MOE_BLOCK) + N_EXPERTS
    n_rows = n_blocks * MOE_BLOCK
    row_tok = jnp.zeros((n_rows,), jnp.int32).at[dest].set(flat_tok[order])
    row_w = jnp.zeros((n_rows,), F32).at[dest].set(flat_w[order])
    blk_expert = jnp.minimum(jnp.searchsorted(pad_end, jnp.arange(n_blocks) * MOE_BLOCK, side='right'), N_EXPERTS - 1)
    xs = t[row_tok].reshape(n_blocks, MOE_BLOCK, d)

    def expert_block(args):
        xb, e = args
        hid = jax.nn.silu(xb @ w1[e]) * (xb @ w3[e])
        return hid @ w2[e]

    ys = lax.map(expert_block, (xs, blk_expert)).reshape(n_rows, d)
    ys = ys * row_w[:, None].astype(ys.dtype)
    out = jax.ops.segment_sum(ys, row_tok, num_segments=n_tok)
    return out.reshape(b, s, d)


def setup_inputs(seed: int = 0) -> dict:
    key = jax.random.key(seed)
    ks = jax.random.split(key, 24)
    L = DEPTH
    nrm = lambda k, shape, scale: jax.random.normal(k, shape, F32) * scale
    gain = lambda k, shape: 1.0 + 0.02 * jax.random.normal(k, shape, F32)
    dt = jnp.exp(jax.random.uniform(ks[6], (L, 2, GDN_HEADS), F32, math.log(1e-3), math.log(1e-1)))
    return {
        'x': nrm(ks[0], (BATCH, SEQ, D_MODEL), 1.0),
        'c': nrm(ks[1], (BATCH, D_MODEL), 1.0),
        'w_ada': nrm(ks[2], (L, D_MODEL, 6 * D_MODEL), 0.5 * D_MODEL ** -0.5),
        'b_ada': nrm(ks[3], (L, 6 * D_MODEL), 0.02),
        'w_in': nrm(ks[4], (L, D_MODEL, IN_COLS), D_MODEL ** -0.5),
        'conv_w': nrm(ks[5], (L, CONV_K, 3 * GDN_WIDTH), CONV_K ** -0.5),
        'a_log': jnp.log(jax.random.uniform(ks[7], (L, 2, GDN_HEADS), F32, 1.0, 16.0)),
        'dt_bias': dt + jnp.log(-jnp.expm1(-dt)),
        'gdn_norm_g': gain(ks[8], (L, HEAD_DIM)),
        'q_norm_g': gain(ks[9], (L, HEAD_DIM)),
        'k_norm_g': gain(ks[10], (L, HEAD_DIM)),
        'attn_norm_g': gain(ks[11], (L, ATTN_WIDTH)),
        'w_out': nrm(ks[12], (L, MIX_WIDTH, D_MODEL), BETA * MIX_WIDTH ** -0.5),
        'ln1_g': gain(ks[13], (L, D_MODEL)),
        'ln1_b': nrm(ks[14], (L, D_MODEL), 0.02),
        'w_group': nrm(ks[15], (L, D_MODEL, N_GROUPS), D_MODEL ** -0.5),
        'b_group': nrm(ks[16], (L, N_GROUPS), 0.01),
        'w_router': nrm(ks[17], (L, D_MODEL, N_EXPERTS), D_MODEL ** -0.5),
        'b_router': nrm(ks[18], (L, N_EXPERTS), 0.01),
        'w1': nrm(ks[19], (L, N_EXPERTS, D_MODEL, D_EXPERT), D_MODEL ** -0.5),
        'w3': nrm(ks[20], (L, N_EXPERTS, D_MODEL, D_EXPERT), D_MODEL ** -0.5),
        'w2': nrm(ks[21], (L, N_EXPERTS, D_EXPERT, D_MODEL), BETA * D_EXPERT ** -0.5),
        'ln2_g': gain(ks[22], (L, D_MODEL)),
        'ln2_b': nrm(ks[23], (L, D_MODEL), 0.02),
    }


def reference(x, c, w_ada, b_ada, w_in, conv_w, a_log, dt_bias, gdn_norm_g, q_norm_g, k_norm_g, attn_norm_g,
              w_out, ln1_g, ln1_b, w_group, b_group, w_router, b_router, w1, w3, w2, ln2_g, ln2_b):
    split_points = np.cumsum(IN_SPLITS)[:-1].tolist()
    for layer in range(DEPTH):
        mod = jax.nn.silu(c) @ w_ada[layer] + b_ada[layer]
        sh1, sc1, gt1, sh2, sc2, gt2 = jnp.split(mod[:, None, :], 6, axis=-1)
        h = layer_norm(x) * (1.0 + sc1) + sh1
        proj = h @ w_in[layer]
        g_q, g_k, g_v, g_z, g_ab, a_q, a_k, a_v = jnp.split(proj, split_points, axis=-1)
        gdn_out = gdn_mixer(g_q, g_k, g_v, g_z, g_ab, conv_w[layer], a_log[layer], dt_bias[layer], gdn_norm_g[layer])
        attn_out = attention_mixer(a_q, a_k, a_v, q_norm_g[layer], k_norm_g[layer], attn_norm_g[layer])
        mixed = jnp.concatenate([gdn_out, attn_out], axis=-1) @ w_out[layer]
        x = layer_norm(ALPHA * x + gt1 * mixed, ln1_g[layer], ln1_b[layer])
        h = layer_norm(x) * (1.0 + sc2) + sh2
        ffn = hierarchical_moe(h, w_group[layer], b_group[layer], w_router[layer], b_router[layer],
                               w1[layer], w3[layer], w2[layer])
        x = layer_norm(ALPHA * x + gt2 * ffn, ln2_g[layer], ln2_b[layer])
    return x
```

```python
import contextlib
import numpy as np
import concourse.bass as bass
import concourse.mybir as mybir
from concourse.bass_utils import run_bass_kernel_spmd

F32 = mybir.dt.float32
BF16 = mybir.dt.bfloat16
AF = mybir.ActivationFunctionType
ALU = mybir.AluOpType
AX = mybir.AxisListType

D = 1024
S = 8192
OWN = 4096
NT = S // 128
NTO = OWN // 128
LN_EPS = 1e-5
RMS_EPS = 1e-6
ALPHA = 2.0 ** 0.25
STAGE = 99
SKIP_GDN = False
SAME_ENGINE_WAIT = True
DEBUG = False


class Buf:
    __slots__ = ("w", "r", "excl")

    def __init__(self):
        self.w = None
        self.r = {}
        self.excl = False


class Stream:
    def __init__(self, h):
        self.h = h
        self.seen = {}


class Eng:
    def __init__(self, name, stream, sems, dma=False):
        self.name = name
        self.stream = stream
        self.sems = sems
        self.dma = dma
        self.count = 0
        self.uses = [0] * len(sems)
        self.rr = 0


class TL:
    def __init__(self, t):
        self.t = t
        self.b = Buf()

    def __getitem__(self, k):
        return self.t[k]


class KB:
    def __init__(self, nc, es):
        self.nc = nc
        self.es = es
        mk = lambda n: es.enter_context(nc.semaphore(n))
        self.s_pe = Stream(nc.tensor)
        self.s_act = Stream(nc.scalar)
        self.s_dve = Stream(nc.vector)
        self.s_pool = Stream(nc.gpsimd)
        self.s_sp = Stream(nc.sync)
        self.pe = Eng("pe", self.s_pe, [mk("s_pe")])
        self.act = Eng("act", self.s_act, [mk("s_act")])
        self.dve = Eng("dve", self.s_dve, [mk("s_dve")])
        self.pool = Eng("pool", self.s_pool, [mk("s_pool")])
        self.ld = Eng("ld", self.s_sp, [mk(f"s_ld{i}") for i in range(8)], dma=True)
        self.st = Eng("st", self.s_pool, [mk(f"s_st{i}") for i in range(6)], dma=True)
        self.n_inst = 0

    def _wait(self, st, tok, eng=None):
        if tok is None:
            return
        sem, val = tok
        key = id(sem)
        if st.seen.get(key, 0) >= val:
            return
        if eng is not None and (not SAME_ENGINE_WAIT or eng.name == "pe") and not eng.dma and sem is eng.sems[0]:
            return
        st.h.wait_ge(sem, val)
        st.seen[key] = val

    def op(self, eng, fn, R=(), W=(), sig=True):
        st = eng.stream
        R = [x.b if isinstance(x, TL) else x for x in R]
        W = [x.b if isinstance(x, TL) else x for x in W]
        W = W + [b for b in R if b.excl and b not in W]
        R = [b for b in R if not b.excl]
        need = {}

        def add(t):
            if t is None:
                return
            k = id(t[0])
            if k not in need or need[k][1] < t[1]:
                need[k] = t

        for b in R:
            add(b.w)
        for b in W:
            add(b.w)
            for t in b.r.values():
                add(t)
        for t in need.values():
            self._wait(st, t, eng)
        if eng.dma:
            i = eng.rr
            eng.rr = (eng.rr + 1) % len(eng.sems)
            sem = eng.sems[i]
            if eng.uses[i] > 0:
                self._wait(st, (sem, 16 * eng.uses[i]))
            inst = fn(st.h)
            eng.uses[i] += 1
            inst.then_inc(sem, 16)
            tok = (sem, 16 * eng.uses[i])
        else:
            inst = fn(st.h)
            sem = eng.sems[0]
            tok = (sem, eng.count + 1)
            if sig:
                eng.count += 1
                inst.then_inc(sem, 1)
        self.n_inst += 1
        for b in R:
            k = id(tok[0])
            if k not in b.r or b.r[k][1] < tok[1]:
                b.r[k] = tok
        for b in W:
            b.w = tok
            b.r = {}
        return tok

    def dma(self, q, out, in_, R=(), W=()):
        return self.op(q, lambda h: h.dma_start(out=out, in_=in_), R, W)

    def mm(self, out, pairs, R=(), W=(), transpose=False):
        n = len(pairs)
        for i, (l, r) in enumerate(pairs):
            self.op(self.pe,
                    lambda h, l=l, r=r, i=i: h.matmul(out, l, r, start=(i == 0), stop=(i == n - 1)),
                    R, W, sig=(i == n - 1))

    def tr(self, out, in_, ident, R=(), W=()):
        self.op(self.pe, lambda h: h.transpose(out, in_, ident), R, W)

    def actf(self, out, in_, func, R=(), W=(), **kw):
        self.op(self.act, lambda h: h.activation(out=out, in_=in_, func=func, **kw), R, W)

    def ts(self, eng, out, in0, s1, s2, op0, op1=None, R=(), W=(), **kw):
        if op1 is None:
            self.op(eng, lambda h: h.tensor_scalar(out=out, in0=in0, scalar1=s1, scalar2=None, op0=op0, **kw), R, W)
        else:
            self.op(eng, lambda h: h.tensor_scalar(out=out, in0=in0, scalar1=s1, scalar2=s2, op0=op0, op1=op1, **kw), R, W)

    def tt(self, eng, out, in0, in1, op, R=(), W=()):
        self.op(eng, lambda h: h.tensor_tensor(out=out, in0=in0, in1=in1, op=op), R, W)

    def stt(self, out, in0, scalar, in1, op0, op1, R=(), W=()):
        self.op(self.dve, lambda h: h.scalar_tensor_tensor(out=out, in0=in0, scalar=scalar, in1=in1, op0=op0, op1=op1), R, W)

    def cp(self, eng, out, in_, R=(), W=()):
        if eng is self.act:
            self.op(eng, lambda h: h.copy(out=out, in_=in_), R, W)
        else:
            self.op(eng, lambda h: h.tensor_copy(out=out, in_=in_), R, W)

    def barrier(self):
        toks = []
        for e in (self.pe, self.act, self.dve, self.pool):
            if e.count > 0:
                toks.append((e.sems[0], e.count))
        for q in (self.st, self.ld):
            for i, sem in enumerate(q.sems):
                if q.uses[i] > 0:
                    toks.append((sem, 16 * q.uses[i]))
        for st in (self.s_pe, self.s_act, self.s_dve, self.s_pool, self.s_sp):
            for t in toks:
                self._wait(st, t)

    def final_wait(self):
        for q in (self.st, self.ld):
            for i, sem in enumerate(q.sems):
                if q.uses[i] > 0:
                    self._wait(self.s_sp, (sem, 16 * q.uses[i]))


class Phase:
    def __init__(self, kb):
        self.kb = kb
        self.es = contextlib.ExitStack()
        self.n = 0

    def __enter__(self):
        self.es.__enter__()
        return self

    def __exit__(self, *a):
        self.kb.barrier()
        return self.es.__exit__(*a)

    def sb(self, name, shape, dt):
        KB.uid = getattr(KB, "uid", 0) + 1
        return TL(self.es.enter_context(self.kb.nc.sbuf_tensor(f"sb{KB.uid}_{name}", shape, dt)))

    def ps(self, name, shape, dt):
        KB.uid = getattr(KB, "uid", 0) + 1
        t = TL(self.es.enter_context(self.kb.nc.psum_tensor(f"ps{KB.uid}_{name}", shape, dt)))
        t.b.excl = True
        return t


class LNS:
    def __init__(self, ph, tag, n=3):
        n = max(n, 3)
        self.sets = [tuple(ph.sb(f"{tag}{nm}{i}", shp, F32) for nm, shp in (("stt", [128, 12]), ("mv", [128, 2]), ("rstd", [128, 1]), ("nmr", [128, 1])))
                     for i in range(n)]
        self.i = 0

    def next(self):
        self.i += 1
        return self.sets[self.i % len(self.sets)]


def ln_stats(kb, ph, xt, stt_, mv, rstd, nmr, tag):
    for c in range(2):
        kb.op(kb.dve, lambda h, c=c: h.bn_stats(out=stt_[:, c * 6:(c + 1) * 6], in_=xt[:, c * 512:(c + 1) * 512]), [xt], [stt_])
    kb.op(kb.dve, lambda h: h.bn_aggr(out=mv[:, 0:2], in_=stt_[:, 0:12]), [stt_], [mv])
    kb.ts(kb.dve, rstd[:, 0:1], mv[:, 1:2], LN_EPS, None, ALU.add, R=[mv], W=[rstd])
    kb.actf(rstd[:, 0:1], rstd[:, 0:1], AF.Sqrt, R=[rstd], W=[rstd])
    kb.op(kb.dve, lambda h: h.reciprocal(out=rstd[:, 0:1], in_=rstd[:, 0:1]), [rstd], [rstd])
    kb.stt(nmr[:, 0:1], mv[:, 0:1], -1.0, rstd[:, 0:1], ALU.mult, ALU.mult, R=[mv, rstd], W=[nmr])


def const_col(kb, ph, name, val):
    t = ph.sb(name, [128, 1], F32)
    kb.op(kb.dve, lambda h: h.memset(t[:], float(val)), [], [t])
    return t


def attention_phase(kb, ph, nc, A, ident, identb, projT, projT_b, catT, catT_b):
    SC = 128.0 ** -0.5
    KT = ph.sb("KT", [128, 2, S], BF16)
    VT = ph.sb("VT", [128, NT, 2, 128], BF16)
    QT = ph.sb("QT", [128, 4, OWN], BF16)
    onesb = ph.sb("onesb", [128, 128], BF16)
    pmf = ph.sb("pmf", [128, 128], F32)
    pmb = ph.sb("pmb", [128, 128], BF16)
    qg = ph.sb("qg", [128, 1], F32)
    kg = ph.sb("kg", [128, 1], F32)
    ang = ph.sb("ang", [128, 4], F32)
    eps1 = const_col(kb, ph, "eps1", RMS_EPS)
    kb.op(kb.dve, lambda h: h.memset(onesb[:], 1.0), [], [onesb])
    kb.dma(kb.ld, pmf[:], A["pm_d"][:, :], W=[pmf])
    kb.cp(kb.dve, pmb[:], pmf[:], R=[pmf], W=[pmb])
    kb.dma(kb.ld, qg[:], A["qg"][:, :], W=[qg])
    kb.dma(kb.ld, kg[:], A["kg"][:, :], W=[kg])
    kb.dma(kb.ld, ang[:], A["ang"][:, :], W=[ang])
    xin = [ph.sb(f"axin{i}", [128, 512], BF16) for i in range(3)]
    cs_ = [ph.sb(f"acs{i}", [128, 512], F32) for i in range(2)]
    sn_ = [ph.sb(f"asn{i}", [128, 512], F32) for i in range(2)]
    sq = ph.sb("asq", [128, 512], BF16)
    rs = ph.sb("ars", [128, 512], F32)
    xnb = ph.sb("axnb", [128, 512], BF16)
    t1 = ph.sb("at1", [128, 512], F32)
    t2 = ph.sb("at2", [128, 512], F32)
    pS = [ph.ps(f"apS{i}", [128, 512], F32) for i in range(3)]
    pO = [ph.ps(f"apO{i}", [128, 512], F32) for i in range(2)]
    pL = [ph.ps(f"apL{i}", [128, 512], F32) for i in range(2)]
    pVt = ph.ps("apV", [128, 512], BF16)
    li = 0
    for tb in range(S // 512):
        cc, ss_ = cs_[tb % 2], sn_[tb % 2]
        kb.dma(kb.ld, cc[:], A["cosT"][:, tb * 512:(tb + 1) * 512], W=[cc])
        kb.dma(kb.ld, ss_[:], A["sinT"][:, tb * 512:(tb + 1) * 512], W=[ss_])
        jobs = [("k", 0), ("k", 1)]
        if tb < OWN // 512:
            jobs += [("q", i) for i in range(4)]
        for kind, hh in jobs:
            xi = xin[li % 3]
            li += 1
            row0 = (1024 + hh * 128) if kind == "k" else (2560 + hh * 128)
            kb.dma(kb.ld, xi[:], projT[row0:row0 + 128, tb * 512:(tb + 1) * 512], R=[projT_b], W=[xi])
            kb.actf(sq[:], xi[:], AF.Square, R=[xi], W=[sq])
            p_ = pS[0]
            kb.mm(p_[:, :], [(onesb[:], sq[:])], R=[onesb, sq], W=[p_])
            kb.actf(rs[:], p_[:, :], AF.Sqrt, R=[p_, eps1], W=[rs], scale=1.0 / 128.0, bias=eps1[:, 0:1])
            kb.op(kb.dve, lambda h: h.reciprocal(out=rs[:], in_=rs[:]), [rs], [rs])
            g_ = kg if kind == "k" else qg
            kb.stt(xnb[:], xi[:], g_[:, 0:1], rs[:], ALU.mult, ALU.mult, R=[xi, g_, rs], W=[xnb])
            p2 = pS[1]
            kb.mm(p2[:, :], [(pmb[:], xnb[:])], R=[pmb, xnb], W=[p2])
            kb.tt(kb.dve, t1[:], xnb[:], cc[:], ALU.mult, R=[xnb, cc], W=[t1])
            kb.tt(kb.dve, t2[:], p2[:, :], ss_[:], ALU.mult, R=[p2, ss_], W=[t2])
            if kind == "k":
                kb.tt(kb.pool, KT[:, hh, tb * 512:(tb + 1) * 512], t1[:], t2[:], ALU.add, R=[t1, t2], W=[KT])
            else:
                kb.tt(kb.pool, QT[:, hh, tb * 512:(tb + 1) * 512], t1[:], t2[:], ALU.add, R=[t1, t2], W=[QT])
        for hh in range(2):
            xi = xin[li % 3]
            li += 1
            row0 = 1280 + hh * 128
            kb.dma(kb.ld, xi[:], projT[row0:row0 + 128, tb * 512:(tb + 1) * 512], R=[projT_b], W=[xi])
            for f in range(4):
                kb.tr(pVt[:, f * 128:(f + 1) * 128], xi[:, f * 128:(f + 1) * 128], identb[:], R=[xi, identb], W=[pVt])
            for f in range(4):
                kb.cp(kb.act, VT[:, tb * 4 + f, hh, :], pVt[:, f * 128:(f + 1) * 128], R=[pVt], W=[VT])
    pt = [ph.sb(f"apt{i}", [128, 512], BF16) for i in range(3)]
    araw = ph.sb("araw", [128, 4, 512], F32)
    accD = ph.sb("aaccD", [128, 512], F32)
    accP = ph.sb("aaccP", [128, 512], F32)
    accD1 = ph.sb("aaccD1", [128, 512], F32)
    ones32a = ph.sb("aones32", [128, 128], F32)
    kb.op(kb.dve, lambda h: h.memset(ones32a[:], 1.0), [], [ones32a])
    rl = ph.sb("arl", [128, 512], F32)
    aout = [ph.sb(f"aout{i}", [128, 512], BF16) for i in range(2)]
    gi = 0
    ao = 0
    for qb in range(OWN // 512):
        for qh in range(4):
            kvh = qh // 2
            po, pl = pO[gi % 2], pL[gi % 2]
            gi += 1
            qsl = QT[:, qh, qb * 512:(qb + 1) * 512]

            def s_mm(kt):
                p_ = pS[kt % 3]
                kb.mm(p_[:, :], [(KT[:, kvh, kt * 128:(kt + 1) * 128], qsl)], R=[KT, QT], W=[p_])

            def e_pv(kt):
                p_ = pS[kt % 3]
                e_ = pt[kt % 3]
                kb.actf(e_[:], p_[:, :], AF.Exp, R=[p_], W=[e_], scale=SC)
                kb.op(kb.pe, lambda h: h.matmul(po[:, :], VT[:, kt, kvh, :], e_[:], start=(kt == 0), stop=(kt == NT - 1)),
                      [VT, e_], [po])
                m4 = kt % 4
                if m4 == 1:
                    kb.op(kb.pe, lambda h: h.matmul(pl[:, :], onesb[:], e_[:], start=(kt == 1), stop=False), [onesb, e_], [pl])
                elif m4 == 3:
                    if kt == 3:
                        kb.cp(kb.pool, accP[:], e_[:], R=[e_], W=[accP])
                    else:
                        kb.tt(kb.pool, accP[:], accP[:], e_[:], ALU.add, R=[accP, e_], W=[accP])
                else:
                    ad = accD if m4 == 0 else accD1
                    if kt == m4:
                        kb.cp(kb.dve, ad[:], e_[:], R=[e_], W=[ad])
                    else:
                        kb.tt(kb.dve, ad[:], ad[:], e_[:], ALU.add, R=[ad, e_], W=[ad])

            s_mm(0)
            for kt in range(NT):
                if kt + 1 < NT:
                    s_mm(kt + 1)
                e_pv(kt)
            for ai, a_ in enumerate((accD, accD1, accP)):
                kb.op(kb.pe, lambda h, a_=a_, ai=ai: h.matmul(pl[:, :], ones32a[:], a_[:], start=False, stop=(ai == 2)),
                      [ones32a, a_], [pl])
            kb.op(kb.dve, lambda h: h.reciprocal(out=rl[:], in_=pl[:, :]), [pl], [rl])
            kb.tt(kb.dve, araw[:, qh, :], po[:, :], rl[:], ALU.mult, R=[po, rl], W=[araw])
        pn = pS[0]
        for qh in range(4):
            kb.actf(sq[:], araw[:, qh, :], AF.Square, R=[araw], W=[sq])
            kb.op(kb.pe, lambda h, qh=qh: h.matmul(pn[:, :], onesb[:], sq[:], start=(qh == 0), stop=(qh == 3)), [onesb, sq], [pn])
        kb.actf(rs[:], pn[:, :], AF.Sqrt, R=[pn, eps1], W=[rs], scale=1.0 / 512.0, bias=eps1[:, 0:1])
        kb.op(kb.dve, lambda h: h.reciprocal(out=rs[:], in_=rs[:]), [rs], [rs])
        for qh in range(4):
            o_ = aout[ao % 2]
            ao += 1
            kb.stt(o_[:], araw[:, qh, :], ang[:, qh:qh + 1], rs[:], ALU.mult, ALU.mult, R=[araw, ang, rs], W=[o_])
            kb.dma(kb.st, catT[512 + qh * 128:512 + (qh + 1) * 128, qb * 512:(qb + 1) * 512], o_[:], R=[o_])


def rep_from_cols(kb, ph, ident, cols, dst, pX, dg, ones32):
    for c in range(8):
        kb.ts(kb.dve, dg[:], ident[:], cols[:, c:c + 1], None, ALU.mult, R=[ident, cols], W=[dg])
        kb.mm(pX[:, (c % 4) * 128:(c % 4 + 1) * 128], [(ones32[:], dg[:])], R=[ones32, dg], W=[pX])
        if c % 4 == 3:
            kb.cp(kb.dve, dst[:, (c - 3) * 128:(c + 1) * 128], pX[:, :], R=[pX], W=[dst])


def wout_phase(kb, ph, nc, A, ident, identb, modT, sc2p, catT, catT_b, x, x1s, x1s_b, h2Ts, h2Ts_b, wt_all, gt2rep):
    wob = ph.sb("wob", [128, 8, D], BF16)
    wst = [ph.sb(f"wost{i}", [128, D], F32) for i in range(2)]
    wv = A["w_out"].rearrange("(k p) n -> p k n", p=128)
    for k in range(8):
        w = wst[k % 2]
        kb.dma(kb.ld, w[:], wv[:, k, :], W=[w])
        kb.cp(kb.pool, wob[:, k, :], w[:], R=[w], W=[wob])
    ones32 = ph.sb("ones32", [128, 128], F32)
    kb.op(kb.dve, lambda h: h.memset(ones32[:], 1.0), [], [ones32])
    dg = ph.sb("dg", [128, 128], F32)
    gt1rep = ph.sb("gt1rep", [128, D], F32)
    pX = ph.ps("pX", [128, 512], F32)
    gt1c = ph.sb("gt1c", [128, 8], F32)
    gt2c = ph.sb("gt2c", [128, 8], F32)
    kb.cp(kb.dve, gt1c[:], modT[:, 16:24], R=[modT], W=[gt1c])
    kb.cp(kb.dve, gt2c[:], modT[:, 40:48], R=[modT], W=[gt2c])
    rep_from_cols(kb, ph, ident, gt1c, gt1rep, pX, dg, ones32)
    rep_from_cols(kb, ph, ident, gt2c, gt2rep, pX, dg, ones32)
    g1 = ph.sb("ln1g", [128, D], F32)
    b1 = ph.sb("ln1b", [128, D], F32)
    kb.dma(kb.ld, g1[:], A["ln1g"][:, :], W=[g1])
    kb.dma(kb.ld, b1[:], A["ln1b"][:, :], W=[b1])
    wr32 = ph.sb("wr32", [128, 8, 36], F32)
    kb.dma(kb.ld, wr32[:], A["wr"].rearrange("(k p) n -> p k n", p=128), W=[wr32])
    br = ph.sb("br", [128, 36], F32)
    kb.dma(kb.ld, br[:], A["br"][:, :], W=[br])
    lns = LNS(ph, "w", 6)
    pM = [ph.ps(f"wpM{i}", [128, 512], F32) for i in range(2)]
    pT_ = [ph.ps(f"wpT{i}", [128, 512], F32) for i in range(2)]
    pR = ph.ps("wpR", [128, 36], F32)

    class WS:
        def __init__(w, i):
            w.cat = ph.sb(f"cat{i}", [128, 8, 128], BF16)
            w.xt = ph.sb(f"wxt{i}", [128, D], F32)
            w.tmp = ph.sb(f"wtmp{i}", [128, D], F32)
            w.x1 = ph.sb(f"wx1{i}", [128, D], F32)
            w.xn2 = ph.sb(f"wxn2{i}", [128, D], F32)
            w.h2f = ph.sb(f"h2f{i}", [128, 8, 128], F32)
            w.h2b = ph.sb(f"h2b{i}", [128, 8, 128], BF16)
            w.lg = ph.sb(f"lg{i}", [128, 36], F32)
            w.sm = ph.sb(f"rsm{i}", [128, 16], F32)
            w.ohg, w.mb, w.eg = [ph.sb(f"r4{j}_{i}", [128, 4], F32) for j in range(3)]
            w.ml, w.ml2, w.oh1, w.oh2 = [ph.sb(f"r32{j}_{i}", [128, 32], F32) for j in range(4)]

    catv = catT.rearrange("(k p) t -> p k t", p=128)
    h2v = h2Ts.rearrange("(k p) t -> p k t", p=128)
    xv = x.rearrange("(n p) d -> n p d", p=128)
    x1v = x1s.rearrange("(n p) d -> n p d", p=128)

    def tile_gen(ti, w):
        c_, xx, tmp, xo, xn2, h2f, hb = w.cat, w.xt, w.tmp, w.x1, w.xn2, w.h2f, w.h2b
        lg, sm, ohg, mb, eg, ml, ml2, oh1, oh2 = w.lg, w.sm, w.ohg, w.mb, w.eg, w.ml, w.ml2, w.oh1, w.oh2
        kb.dma(kb.ld, c_[:], catv[:, :, ti * 128:(ti + 1) * 128], W=[c_])
        kb.dma(kb.ld, xx[:], xv[ti, :, :], W=[xx])
        yield
        for nb in range(2):
            kb.mm(pM[nb][:, :], [(c_[:, k, :], wob[:, k, nb * 512:(nb + 1) * 512]) for k in range(8)], R=[c_, wob], W=[pM[nb]])
            kb.tt(kb.dve, tmp[:, nb * 512:(nb + 1) * 512], pM[nb][:, :], gt1rep[:, nb * 512:(nb + 1) * 512], ALU.mult,
                  R=[pM[nb], gt1rep], W=[tmp])
        kb.stt(tmp[:], xx[:], ALPHA, tmp[:], ALU.mult, ALU.add, R=[xx, tmp], W=[tmp])
        yield
        stt_, mv, rstd, nmr = lns.next()
        ln_stats(kb, ph, tmp, stt_, mv, rstd, nmr, "e1")
        yield
        kb.actf(xo[:], tmp[:], AF.Identity, R=[tmp, rstd, nmr], W=[xo], scale=rstd[:, 0:1], bias=nmr[:, 0:1])
        kb.tt(kb.pool, xo[:], xo[:], g1[:], ALU.mult, R=[xo, g1], W=[xo])
        kb.tt(kb.pool, xo[:], xo[:], b1[:], ALU.add, R=[xo, b1], W=[xo])
        kb.dma(kb.st, x1v[ti, :, :], xo[:], R=[xo])
        yield
        stt_, mv, rstd, nmr = lns.next()
        ln_stats(kb, ph, xo, stt_, mv, rstd, nmr, "e2")
        yield
        kb.actf(xn2[:], xo[:], AF.Identity, R=[xo, rstd, nmr], W=[xn2], scale=rstd[:, 0:1], bias=nmr[:, 0:1])
        yield
        for half in range(2):
            p_ = pT_[half]
            for f in range(4):
                kb.tr(p_[:, f * 128:(f + 1) * 128], xn2[:, (half * 4 + f) * 128:(half * 4 + f + 1) * 128], ident[:],
                      R=[xn2, ident], W=[p_])
            for f in range(4):
                fc = half * 4 + f
                kb.actf(h2f[:, fc, :], p_[:, f * 128:(f + 1) * 128], AF.Identity,
                        R=[p_, sc2p, modT], W=[h2f], scale=sc2p[:, fc:fc + 1], bias=modT[:, 24 + fc:25 + fc])
            yield
        kb.cp(kb.pool, hb[:], h2f[:], R=[h2f], W=[hb])
        kb.dma(kb.st, h2v[:, :, ti * 128:(ti + 1) * 128], hb[:], R=[hb])
        kb.mm(pR[:, :], [(h2f[:, k, :], wr32[:, k, :]) for k in range(8)], R=[h2f, wr32], W=[pR])
        kb.tt(kb.dve, lg[:], pR[:, :], br[:], ALU.add, R=[pR, br], W=[lg])
        yield
        dv = kb.dve
        kb.op(dv, lambda h: h.tensor_reduce(out=sm[:, 0:1], in_=lg[:, 0:4], axis=AX.X, op=ALU.max), [lg], [sm])
        kb.ts(dv, ohg[:], lg[:, 0:4], sm[:, 0:1], None, ALU.is_equal, R=[lg, sm], W=[ohg])
        kb.ts(dv, sm[:, 1:2], sm[:, 0:1], -1.0, None, ALU.mult, R=[sm], W=[sm])
        yield
        kb.actf(eg[:], lg[:, 0:4], AF.Exp, R=[lg, sm], W=[eg, sm], bias=sm[:, 1:2], accum_out=sm[:, 2:3])
        kb.op(dv, lambda h: h.reciprocal(out=sm[:, 3:4], in_=sm[:, 2:3]), [sm], [sm])
        kb.ts(dv, mb[:], ohg[:], -1.0, 1e30, ALU.add, ALU.mult, R=[ohg], W=[mb])
        yield
        for g in range(4):
            kb.ts(dv, ml[:, g * 8:(g + 1) * 8], lg[:, 4 + g * 8:12 + g * 8], mb[:, g:g + 1], None, ALU.add, R=[lg, mb], W=[ml])
        yield
        kb.op(dv, lambda h: h.tensor_reduce(out=sm[:, 4:5], in_=ml[:], axis=AX.X, op=ALU.max), [ml], [sm])
        kb.ts(dv, oh1[:], ml[:], sm[:, 4:5], None, ALU.is_equal, R=[ml, sm], W=[oh1])
        yield
        kb.stt(ml2[:], oh1[:], -1e30, ml[:], ALU.mult, ALU.add, R=[oh1, ml], W=[ml2])
        kb.op(dv, lambda h: h.tensor_reduce(out=sm[:, 5:6], in_=ml2[:], axis=AX.X, op=ALU.max), [ml2], [sm])
        yield
        kb.ts(dv, oh2[:], ml2[:], sm[:, 5:6], None, ALU.is_equal, R=[ml2, sm], W=[oh2])
        kb.ts(dv, sm[:, 6:7], sm[:, 4:5], -1.0, None, ALU.mult, R=[sm], W=[sm])
        yield
        kb.actf(sm[:, 7:8], sm[:, 5:6], AF.Exp, R=[sm], W=[sm], bias=sm[:, 6:7])
        kb.ts(dv, sm[:, 8:9], sm[:, 7:8], 1.0, None, ALU.add, R=[sm], W=[sm])
        yield
        kb.op(dv, lambda h: h.reciprocal(out=sm[:, 9:10], in_=sm[:, 8:9]), [sm], [sm])
        kb.tt(dv, sm[:, 10:11], sm[:, 9:10], sm[:, 3:4], ALU.mult, R=[sm], W=[sm])
        yield
        kb.tt(dv, sm[:, 11:12], sm[:, 10:11], sm[:, 7:8], ALU.mult, R=[sm], W=[sm])
        kb.ts(dv, oh1[:], oh1[:], sm[:, 10:11], None, ALU.mult, R=[oh1, sm], W=[oh1])
        yield
        kb.stt(wt_all[:, ti, :], oh2[:], sm[:, 11:12], oh1[:], ALU.mult, ALU.add, R=[oh2, sm, oh1], W=[wt_all])

    NSL = 3
    slots = [WS(i) for i in range(NSL)]
    active = []
    nxt_ti = 0
    tick = 0
    while active or nxt_ti < NTO:
        if nxt_ti < NTO and len(active) < NSL and tick % 6 == 0:
            active.append(tile_gen(nxt_ti, slots[nxt_ti % NSL]))
            nxt_ti += 1
        tick += 1
        still = []
        for g_ in active:
            try:
                next(g_)
                still.append(g_)
            except StopIteration:
                pass
        active = still
    if DEBUG:
        kb.dma(kb.st, A["wtd"][:, :], wt_all[:].rearrange("p a b -> p (a b)"), R=[wt_all])


def moe_phase(kb, ph, nc, A, x1s, x1s_b, h2Ts, h2Ts_b, wt_all, gt2rep, y):
    NSB = 4
    TPS = NTO // NSB
    h2 = ph.sb("mh2", [128, 8, TPS * 128], BF16)
    acc = ph.sb("macc", [128, TPS, D], F32)
    st13 = [ph.sb(f"mst13{i}", [128, 8, 256], F32) for i in range(2)]
    st2 = [ph.sb(f"mst2{i}", [128, 2, D], F32) for i in range(2)]
    w1b = [ph.sb(f"mw1b{i}", [128, 8, 512], BF16) for i in range(2)]
    w3b = [ph.sb(f"mw3b{i}", [128, 8, 512], BF16) for i in range(2)]
    w2b = [ph.sb(f"mw2b{i}", [128, 4, D], BF16) for i in range(2)]
    sA = [ph.sb(f"msA{i}", [128, 512], F32) for i in range(3)]
    hid = [ph.sb(f"mhid{i}", [128, 512], BF16) for i in range(3)]
    hidT = [ph.sb(f"mhidT{i}", [128, 4, 128], BF16) for i in range(3)]
    sic = [0]
    pA = [ph.ps(f"mpA{i}", [128, 512], F32) for i in range(2)]
    pB = [ph.ps(f"mpB{i}", [128, 512], F32) for i in range(2)]
    pTr = [ph.ps(f"mpTr{i}", [128, 512], BF16) for i in range(2)]
    pO = [ph.ps(f"mpO{i}", [128, 512], F32) for i in range(2)]
    identb = ph.sb("midb", [128, 128], BF16)
    idf = ph.sb("midf", [128, 128], F32)
    kb.dma(kb.ld, idf[:], A["ident"][:, :], W=[idf])
    kb.cp(kb.dve, identb[:], idf[:], R=[idf], W=[identb])
    g2 = ph.sb("ln2g", [128, D], F32)
    b2 = ph.sb("ln2b", [128, D], F32)
    kb.dma(kb.ld, g2[:], A["ln2g"][:, :], W=[g2])
    kb.dma(kb.ld, b2[:], A["ln2b"][:, :], W=[b2])
    xt = [ph.sb(f"mxt{i}", [128, D], F32) for i in range(2)]
    yo = [ph.sb(f"myo{i}", [128, D], F32) for i in range(2)]
    lns = LNS(ph, "m")
    h2v = h2Ts.rearrange("(k p) t -> p k t", p=128)
    x1v = x1s.rearrange("(n p) d -> n p d", p=128)
    yv = y.rearrange("(n p) d -> n p d", p=128)
    si = 0
    it = 0
    for sbk in range(NSB):
        for k in range(8):
            kb.dma(kb.ld, h2[:, k, :], h2v[:, k, sbk * TPS * 128:(sbk + 1) * TPS * 128], R=[h2Ts_b], W=[h2])
        def load_pair(ep):
            wa, wc, wd = w1b[ep % 2], w3b[ep % 2], w2b[ep % 2]
            for j in range(2):
                e = ep * 2 + j
                for (src_, dst) in ((A["w1"], wa), (A["w3"], wc)):
                    s_ = st13[sic[0] % 2]
                    sic[0] += 1
                    kb.dma(kb.ld, s_[:], src_[e].rearrange("(k p) n -> p k n", p=128), W=[s_])
                    kb.cp(kb.pool if src_ is A["w1"] else kb.act, dst[:, :, j * 256:(j + 1) * 256], s_[:], R=[s_], W=[dst])
                s2 = st2[e % 2]
                kb.dma(kb.ld, s2[:], A["w2"][e].rearrange("(c p) n -> p c n", p=128), W=[s2])
                kb.cp(kb.pool if j == 0 else kb.act, wd[:, j * 2:(j + 1) * 2, :], s2[:], R=[s2], W=[wd])

        items = [(ep, tl) for ep in range(16) for tl in range(TPS)]

        def step1(i):
            ep, tl = items[i]
            ti = sbk * TPS + tl
            wa, wc = w1b[ep % 2], w3b[ep % 2]
            a_, b_ = pA[i % 2], pB[i % 2]
            sA_, hid_ = sA[i % 3], hid[i % 3]
            hs = lambda k: h2[:, k, tl * 128:(tl + 1) * 128]
            kb.mm(a_[:, :], [(hs(k), wa[:, k, :]) for k in range(8)], R=[h2, wa], W=[a_])
            kb.mm(b_[:, :], [(hs(k), wc[:, k, :]) for k in range(8)], R=[h2, wc], W=[b_])
            kb.actf(sA_[:], a_[:, :], AF.Silu, R=[a_], W=[sA_])
            for j in range(2):
                e = ep * 2 + j
                kb.stt(hid_[:, j * 256:(j + 1) * 256], sA_[:, j * 256:(j + 1) * 256], wt_all[:, ti, e:e + 1],
                       b_[:, j * 256:(j + 1) * 256], ALU.mult, ALU.mult, R=[sA_, wt_all, b_], W=[hid_])

        def step2(i):
            hid_, hT_, pt_ = hid[i % 3], hidT[i % 3], pTr[i % 2]
            for c in range(4):
                kb.tr(pt_[:, c * 128:(c + 1) * 128], hid_[:, c * 128:(c + 1) * 128], identb[:], R=[hid_, identb], W=[pt_])
            kb.cp(kb.act, hT_[:].rearrange("p c t -> p (c t)"), pt_[:, :], R=[pt_], W=[hT_])

        def step3(i):
            ep, tl = items[i]
            wd = w2b[ep % 2]
            hT_ = hidT[i % 3]
            for nb in range(2):
                o_ = pO[nb]
                kb.mm(o_[:, :], [(hT_[:, c, :], wd[:, c, nb * 512:(nb + 1) * 512]) for c in range(4)], R=[hT_, wd], W=[o_])
                dst = acc[:, tl, nb * 512:(nb + 1) * 512]
                if ep == 0:
                    kb.cp(kb.dve, dst, o_[:, :], R=[o_], W=[acc])
                else:
                    kb.tt(kb.dve, dst, dst, o_[:, :], ALU.add, R=[acc, o_], W=[acc])

        load_pair(0)
        for i in range(len(items) + 2):
            if i < len(items):
                ep, tl = items[i]
                if tl == 2 and ep + 1 < 16:
                    load_pair(ep + 1)
                step1(i)
            if 0 <= i - 1 < len(items):
                step2(i - 1)
            if 0 <= i - 2 < len(items):
                step3(i - 2)
        for tl in range(TPS):
            ti = sbk * TPS + tl
            xx = xt[ti % 2]
            yy = yo[ti % 2]
            kb.dma(kb.ld, xx[:], x1v[ti, :, :], R=[x1s_b], W=[xx])
            kb.tt(kb.dve, acc[:, tl, :], acc[:, tl, :], gt2rep[:], ALU.mult, R=[acc, gt2rep], W=[acc])
            kb.stt(yy[:], xx[:], ALPHA, acc[:, tl, :], ALU.mult, ALU.add, R=[xx, acc], W=[yy])
            stt_, mv, rstd, nmr = lns.next()
            ln_stats(kb, ph, yy, stt_, mv, rstd, nmr, "f")
            kb.actf(yy[:], yy[:], AF.Identity, R=[yy, rstd, nmr], W=[yy], scale=rstd[:, 0:1], bias=nmr[:, 0:1])
            kb.tt(kb.pool, yy[:], yy[:], g2[:], ALU.mult, R=[yy, g2], W=[yy])
            kb.tt(kb.pool, yy[:], yy[:], b2[:], ALU.add, R=[yy, b2], W=[yy])
            kb.dma(kb.st, yv[ti, :, :], yy[:], R=[yy])


def _interleave(gens):
    gens = list(gens)
    while gens:
        nxt_ = []
        for g in gens:
            try:
                next(g)
                nxt_.append(g)
            except StopIteration:
                pass
        gens = nxt_
        yield


def gdn_phase(kb, ph, nc, A, ident, identb, projT, projT_b, abtm, abtm_b, catT, catT_b):
    NM = 6
    mk = ph.sb("gmk", [128, 2 * NM, 128], BF16)
    cu32 = ph.sb("gcu32", [128, 2, 128], F32)
    with Phase(kb) as tph:
        mk32 = tph.sb("gmk32", [128, 2 * NM * 128], F32)
        kb.dma(kb.ld, mk32[:], A["masks"][:, :], W=[mk32])
        kb.cp(kb.pool, mk[:].rearrange("p m j -> p (m j)"), mk32[:], R=[mk32], W=[mk])
        for d_ in range(2):
            kb.cp(kb.dve, cu32[:, d_, :], mk32[:, (d_ * NM + 1) * 128:(d_ * NM + 2) * 128], R=[mk32], W=[cu32])
    mk32 = cu32
    MK = lambda d, m: mk[:, d * NM + m, :].unsqueeze(1).broadcast_to([128, 4, 128])
    CU32 = lambda d: cu32[:, d, :]
    id4 = identb
    ID4 = identb[:].unsqueeze(1).broadcast_to([128, 4, 128])
    V3 = lambda t: t[:].rearrange("p (h j) -> p h j", h=4)
    B3 = lambda ap4: ap4.unsqueeze(2).broadcast_to([128, 4, 128])
    ones32 = ph.sb("gones32", [128, 128], F32)
    onesb = ph.sb("gonesb", [128, 128], BF16)
    kb.op(kb.dve, lambda h: h.memset(ones32[:], 1.0), [], [ones32])
    kb.op(kb.dve, lambda h: h.memset(onesb[:], 1.0), [], [onesb])
    cw = ph.sb("gcw", [128, 60], F32)
    kb.dma(kb.ld, cw[:], A["convw"][:, :], W=[cw])
    dg = ph.sb("gdg", [128, 60, 128], BF16)
    for i in range(60):
        kb.ts(kb.dve, dg[:, i, :], identb[:], cw[:, i:i + 1], None, ALU.mult, R=[identb, cw], W=[dg])
    dtb = ph.sb("gdtb", [128, 8], F32)
    nA = ph.sb("gnA", [128, 8], F32)
    gng = ph.sb("ggng", [128, 1], F32)
    kb.dma(kb.ld, dtb[:], A["dtb"][:, :], W=[dtb])
    kb.dma(kb.ld, nA[:], A["alog"][:, :], W=[nA])
    kb.dma(kb.ld, gng[:], A["gng"][:, :], W=[gng])
    kb.actf(nA[:], nA[:], AF.Exp, R=[nA], W=[nA])
    kb.ts(kb.dve, nA[:], nA[:], -1.0, None, ALU.mult, R=[nA], W=[nA])
    one1 = const_col(kb, ph, "gone1", 1.0)
    epsk = const_col(kb, ph, "gepsk", RMS_EPS)
    epsq = const_col(kb, ph, "gepsq", RMS_EPS * 128.0)
    abt = ph.sb("gabt", [128, NT, 16], F32)
    kb.dma(kb.ld, abt[:].rearrange("p n c -> p (n c)"), abtm[:, :], W=[abt])
    ofd = A["ofd"]
    of_b = [Buf() for _ in range(NTO)]
    pp = [ph.ps(f"gpp{i}", [128, 512], F32) for i in range(6)]
    pb16 = [ph.ps(f"gpb{i}", [128, 512], BF16) for i in range(2)]
    cnt = {"p": 0, "b": 0}

    def nxt():
        cnt["p"] += 1
        return pp[cnt["p"] % 6]

    def nxtb():
        cnt["b"] += 1
        return pb16[cnt["b"] % 2]

    H = lambda t, h: t[:, h * 128:(h + 1) * 128]
    uid = [0]

    def W512(name, dt):
        uid[0] += 1
        return ph.sb(f"{name}{uid[0]}", [128, 512], dt)

    def sm(name, w):
        uid[0] += 1
        return ph.sb(f"{name}{uid[0]}", [128, w], F32)

    def mm4(lt, rt, R):
        p_ = nxt()
        for h in range(4):
            kb.mm(H(p_, h), [(H(lt, h), H(rt, h))], R=R, W=[p_])
        return p_

    class BT:
        def __init__(s):
            s.xin = [ph.sb(f"gxin{uid[0]}_{i}", [128, 516], BF16) for i in range(2)]
            uid[0] += 1
            s.kcT = W512("gkcT", BF16)
            s.sq, s.rs = W512("gsq", BF16), W512("grs", F32)
            s.li = 0

    class BS:
        def __init__(s, bt):
            s.bt = bt
            s.khT = [W512("gkhT", BF16) for _ in range(4)]
            s.qhT = [W512("gqhT", BF16) for _ in range(4)]
            s.vT = [W512("gvT", BF16) for _ in range(4)]

    class PI:
        def __init__(s):
            s.t8 = sm("gt8", 8)
            s.gc4, s.gl4, s.kd4, s.bg4 = [sm("gsm", 4) for _ in range(4)]
            s.gbc = [ph.sb(f"ggbc{uid[0]}_{i}", [128, 128], F32) for i in range(2)]
            uid[0] += 1
            s.dm, s.dmT = W512("gdm", F32), W512("gdmT", F32)
            s.dec, s.decT = W512("gdec", BF16), W512("gdecT", BF16)
            s.M4, s.MTs = W512("gM4", BF16), W512("gMTs", BF16)
            s.A_ = [W512("gA", BF16) for _ in range(2)]
            s.N_ = [W512("gN", BF16) for _ in range(2)]
            s.P_, s.Q_ = W512("gP", BF16), W512("gQ", BF16)
            s.Ml = [W512("gMl", BF16) for _ in range(3)]
            s.Nl = [W512("gNl", BF16) for _ in range(2)]
            s.X_, s.Y_ = s.A_[0], s.A_[1]
            s.kbg, s.bv = s.N_[0], s.N_[1]

    class CS:
        def __init__(s):
            s.g8, s.b8 = sm("gg8", 8), sm("gb8", 8)
            s.egc, s.dl4 = sm("gegc", 4), sm("gdl4", 4)
            s.kdec = W512("gkdec", BF16)
            s.wT4, s.u4, s.qkT4 = W512("gwT4", BF16), W512("gu4", F32), W512("gqkT4", BF16)

    class SS:
        def __init__(s, d):
            s.S32, s.S16 = W512("gS32", F32), W512("gS16", BF16)
            s.vn, s.oa4 = W512("gvn", BF16), W512("goa4", F32)
            if d == 0:
                s.ofs = [W512("gofs", BF16) for _ in range(2)]
            if d == 1:
                s.ofl = W512("gofl", BF16)
                s.o4 = W512("go4", F32)
                s.on4, s.zt, s.sz, s.gout, s.junk = [W512("gfin", BF16) for _ in range(5)]
                s.ss4 = sm("gss4", 4)

    def conv_group(bs, grp, row0, tb, dst):
        xi = bs.bt.xin[bs.bt.li % 2]
        bs.bt.li += 1
        lo, hi = tb * 512 - 2, tb * 512 + 514
        clo, chi = max(lo, 0), min(hi, S)
        if clo != lo or chi != hi:
            kb.op(kb.pool, lambda h: h.memset(xi[:], 0.0), [], [xi])
        kb.dma(kb.ld, xi[:, clo - lo:chi - lo], projT[row0:row0 + 128, clo:chi], W=[xi])
        p_ = nxt()
        kb.mm(p_[:, :], [(dg[:, grp * 5 + j, :], xi[:, j:j + 512]) for j in range(5)], R=[dg, xi], W=[p_])
        kb.actf(dst[:], p_[:, :], AF.Silu, R=[p_], W=[dst])

    def l2n(bs, src, dst, scale, epsc):
        kb.actf(bs.bt.sq[:], src[:], AF.Square, R=[src], W=[bs.bt.sq])
        p_ = nxt()
        kb.mm(p_[:, :], [(onesb[:], bs.bt.sq[:])], R=[onesb, bs.bt.sq], W=[p_])
        kb.actf(bs.bt.rs[:], p_[:, :], AF.Sqrt, R=[p_, epsc], W=[bs.bt.rs], scale=scale, bias=epsc[:, 0:1])
        kb.op(kb.dve, lambda hh: hh.reciprocal(out=bs.bt.rs[:], in_=bs.bt.rs[:]), [bs.bt.rs], [bs.bt.rs])
        kb.tt(kb.dve, dst[:], src[:], bs.bt.rs[:], ALU.mult, R=[src, bs.bt.rs], W=[dst])

    def blockprep(bs, tb):
        own = tb < OWN // 512
        for h in range(4):
            conv_group(bs, h, h * 128, tb, bs.bt.kcT)
            l2n(bs, bs.bt.kcT, bs.khT[h], 1.0, epsk)
            yield
            conv_group(bs, 4 + h, 512 + h * 128, tb, bs.vT[h])
            yield
            if own:
                conv_group(bs, 8 + h, 1536 + h * 128, tb, bs.bt.kcT)
                l2n(bs, bs.bt.kcT, bs.qhT[h], 128.0, epsq)
                yield

    def chunkprep(s, bs, n, c, d, own):
        cs = slice(c * 128, (c + 1) * 128)
        last = 127 if d == 0 else 0
        kb.tt(kb.dve, s.t8[:], abt[:, n, 0:8], dtb[:], ALU.add, R=[abt, dtb], W=[s.t8])
        kb.actf(s.t8[:], s.t8[:], AF.Exp, R=[s.t8], W=[s.t8])
        kb.actf(s.t8[:], s.t8[:], AF.Ln, R=[s.t8, one1], W=[s.t8], bias=one1[:, 0:1])
        kb.tt(kb.dve, s.g8[:], s.t8[:], nA[:], ALU.mult, R=[s.t8, nA], W=[s.g8])
        kb.actf(s.b8[:], abt[:, n, 8:16], AF.Sigmoid, R=[abt], W=[s.b8])
        yield
        gd = s.g8[:, d * 4:(d + 1) * 4]
        bd = s.b8[:, d * 4:(d + 1) * 4]
        pc = nxt()
        kb.mm(pc[:, 0:4], [(CU32(d), gd)], R=[mk32, s.g8], W=[pc])
        kb.cp(kb.dve, s.gc4[:], pc[:, 0:4], R=[pc], W=[s.gc4])
        pG = nxt()
        for h in range(4):
            gb_ = s.gbc[h % 2]
            kb.ts(kb.pool, gb_[:], ones32[:], s.g8[:, d * 4 + h:d * 4 + h + 1], None, ALU.mult, R=[ones32, s.g8], W=[gb_])
            kb.mm(H(pG, h), [(gb_[:], CU32(d))], R=[gb_, mk32], W=[pG])
        G3 = pG[:, :].rearrange("p (h j) -> p h j", h=4)
        kb.tt(kb.dve, V3(s.dmT), G3, B3(s.gc4[:]), ALU.subtract, R=[pG, s.gc4], W=[s.dmT])
        kb.cp(kb.dve, s.gl4[:], G3[:, :, last], R=[pG], W=[s.gl4])
        kb.actf(s.dm[:], s.dmT[:], AF.Relu, R=[s.dmT], W=[s.dm])
        kb.actf(s.dmT[:], s.dmT[:], AF.Relu, R=[s.dmT], W=[s.dmT], scale=-1.0)
        yield
        kb.actf(s.dec[:], s.dm[:], AF.Exp, R=[s.dm], W=[s.dec], scale=-1.0)
        kb.actf(s.decT[:], s.dmT[:], AF.Exp, R=[s.dmT], W=[s.decT], scale=-1.0)
        kb.tt(kb.pool, V3(s.dec), V3(s.dec), MK(d, 0), ALU.mult, R=[s.dec, mk], W=[s.dec])
        kb.tt(kb.pool, V3(s.decT), V3(s.decT), MK(d, 1), ALU.mult, R=[s.decT, mk], W=[s.decT])
        kb.actf(s.egc[:], s.gc4[:], AF.Exp, R=[s.gc4], W=[s.egc])
        kb.actf(s.dl4[:], s.gl4[:], AF.Exp, R=[s.gl4], W=[s.dl4])
        kb.tt(kb.dve, s.kd4[:], s.gl4[:], s.gc4[:], ALU.subtract, R=[s.gl4, s.gc4], W=[s.kd4])
        kb.actf(s.kd4[:], s.kd4[:], AF.Exp, R=[s.kd4], W=[s.kd4])
        kb.tt(kb.dve, s.bg4[:], s.egc[:], bd, ALU.mult, R=[s.egc, s.b8], W=[s.bg4])
        yield
        pK = nxt()
        for h in range(4):
            kb.mm(H(pK, h), [(bs.khT[h][:, cs], bs.khT[h][:, cs])], R=[bs.khT[h]], W=[pK])
        kb.tt(kb.dve, s.dm[:], pK[:, :], s.dec[:], ALU.mult, R=[pK, s.dec], W=[s.dm])
        kb.tt(kb.dve, V3(s.M4), V3(s.dm), B3(bd), ALU.mult, R=[s.dm, s.b8], W=[s.M4])
        yield
        pMT = nxtb()
        for h in range(4):
            kb.tr(H(pMT, h), H(s.M4, h), identb[:], R=[s.M4, identb], W=[pMT])
        kb.cp(kb.act, s.MTs[:], pMT[:, :], R=[pMT], W=[s.MTs])
        kb.tt(kb.pool, V3(s.A_[0]), V3(s.M4), MK(d, 2), ALU.mult, R=[s.M4, mk], W=[s.A_[0]])
        for l in range(3):
            kb.tt(kb.pool, V3(s.Ml[l]), V3(s.M4), MK(d, 3 + l), ALU.mult, R=[s.M4, mk], W=[s.Ml[l]])
        yield
        kb.tt(kb.pool, V3(s.N_[0]), V3(s.MTs), MK(1 - d, 2), ALU.mult, R=[s.MTs, mk], W=[s.N_[0]])
        for l in range(2):
            kb.tt(kb.pool, V3(s.Nl[l]), V3(s.MTs), MK(1 - d, 3 + l), ALU.mult, R=[s.MTs, mk], W=[s.Nl[l]])
        kb.tt(kb.dve, V3(s.P_), V3(s.A_[0]), ID4, ALU.add, R=[s.A_[0], id4], W=[s.P_])
        kb.tt(kb.dve, V3(s.Q_), V3(s.N_[0]), ID4, ALU.add, R=[s.N_[0], id4], W=[s.Q_])
        yield
        A_, N_, P_, Q_, Ml, Nl, X_, Y_ = s.A_, s.N_, s.P_, s.Q_, s.Ml, s.Nl, s.X_, s.Y_
        ca, cn = 0, 0
        for k in range(1, 4):
            pa_ = mm4(N_[cn], A_[ca], [N_[cn], A_[ca]])
            if k < 3:
                pn_ = mm4(A_[ca], N_[cn], [N_[cn], A_[ca]])
            kb.cp(kb.act, A_[1 - ca][:], pa_[:, :], R=[pa_], W=[A_[1 - ca]])
            if k < 3:
                kb.cp(kb.act, N_[1 - cn][:], pn_[:, :], R=[pn_], W=[N_[1 - cn]])
                cn = 1 - cn
            ca = 1 - ca
            yield
            pp_ = mm4(Q_, A_[ca], [Q_, A_[ca]])
            pq_ = mm4(A_[ca], Q_, [Q_, A_[ca]])
            kb.tt(kb.dve, P_[:], P_[:], pp_[:, :], ALU.add, R=[P_, pp_], W=[P_])
            kb.tt(kb.dve, Q_[:], Q_[:], pq_[:, :], ALU.add, R=[Q_, pq_], W=[Q_])
            yield
        for l in range(3):
            if l < 2:
                py = mm4(Nl[l], P_, [Nl[l], P_])
                kb.cp(kb.act, Y_[:], py[:, :], R=[py], W=[Y_])
            px = mm4(Ml[l], Q_, [Ml[l], Q_])
            kb.cp(kb.act, X_[:], px[:, :], R=[px], W=[X_])
            yield
            if l < 2:
                pt_ = mm4(Q_, Y_, [Q_, Y_])
            pu_ = mm4(P_, X_, [P_, X_])
            if l < 2:
                kb.tt(kb.dve, P_[:], P_[:], pt_[:, :], ALU.subtract, R=[P_, pt_], W=[P_])
            kb.tt(kb.dve, Q_[:], Q_[:], pu_[:, :], ALU.subtract, R=[Q_, pu_], W=[Q_])
            yield
        pkT = nxtb()
        for h in range(4):
            kb.tr(H(pkT, h), bs.khT[h][:, cs], identb[:], R=[bs.khT[h], identb], W=[pkT])
        pkT3 = pkT[:, :].rearrange("p (h j) -> p h j", h=4)
        kb.tt(kb.dve, V3(s.kbg), pkT3, B3(s.bg4[:]), ALU.mult, R=[pkT, s.bg4], W=[s.kbg])
        kb.tt(kb.dve, V3(s.kdec), pkT3, B3(s.kd4[:]), ALU.mult, R=[pkT, s.kd4], W=[s.kdec])
        yield
        pvT = nxtb()
        for h in range(4):
            kb.tr(H(pvT, h), bs.vT[h][:, cs], identb[:], R=[bs.vT[h], identb], W=[pvT])
        kb.tt(kb.dve, V3(s.bv), pvT[:, :].rearrange("p (h j) -> p h j", h=4), B3(bd), ALU.mult, R=[pvT, s.b8], W=[s.bv])
        yield
        pw = mm4(s.kbg, Q_, [s.kbg, Q_])
        kb.cp(kb.act, s.wT4[:], pw[:, :], R=[pw], W=[s.wT4])
        pu = mm4(Q_, s.bv, [Q_, s.bv])
        kb.cp(kb.act, s.u4[:], pu[:, :], R=[pu], W=[s.u4])
        yield
        if own:
            pqk = nxt()
            for h in range(4):
                kb.mm(H(pqk, h), [(bs.khT[h][:, cs], bs.qhT[h][:, cs])], R=[bs.khT[h], bs.qhT[h]], W=[pqk])
            kb.tt(kb.dve, s.qkT4[:], pqk[:, :], s.decT[:], ALU.mult, R=[pqk, s.decT], W=[s.qkT4])
            yield

    def scan(s, bs, z, n, c, d, own):
        cs = slice(c * 128, (c + 1) * 128)
        pv_ = mm4(s.wT4, z.S16, [s.wT4, z.S16])
        kb.tt(kb.dve, z.vn[:], s.u4[:], pv_[:, :], ALU.subtract, R=[s.u4, pv_], W=[z.vn])
        yield
        ps_ = mm4(s.kdec, z.vn, [s.kdec, z.vn])
        if own:
            pa2 = nxt()
            for h in range(4):
                kb.mm(H(pa2, h), [(bs.qhT[h][:, cs], H(z.S16, h))], R=[bs.qhT[h], z.S16], W=[pa2])
            pb2 = mm4(s.qkT4, z.vn, [s.qkT4, z.vn])
        kb.tt(kb.dve, V3(z.S32), V3(z.S32), B3(s.dl4[:]), ALU.mult, R=[z.S32, s.dl4], W=[z.S32])
        kb.tt(kb.dve, z.S32[:], z.S32[:], ps_[:, :], ALU.add, R=[z.S32, ps_], W=[z.S32])
        if own:
            kb.tt(kb.dve, V3(z.oa4), pa2[:, :].rearrange("p (h j) -> p h j", h=4), B3(s.egc[:]), ALU.mult, R=[pa2, s.egc], W=[z.oa4])
            if d == 0:
                ofs_ = z.ofs[n % 2]
                kb.tt(kb.dve, ofs_[:], z.oa4[:], pb2[:, :], ALU.add, R=[z.oa4, pb2], W=[ofs_])
                kb.dma(kb.st, ofd[n, :, :], ofs_[:], R=[ofs_], W=[of_b[n]])
            else:
                kb.tt(kb.dve, z.o4[:], z.oa4[:], pb2[:, :], ALU.add, R=[z.oa4, pb2], W=[z.o4])
        kb.cp(kb.act, z.S16[:], z.S32[:], R=[z.S32], W=[z.S16])
        yield
        if own and d == 1:
            kb.dma(kb.ld, z.ofl[:], ofd[n, :, :], R=[of_b[n]], W=[z.ofl])
            kb.tt(kb.pool, z.o4[:], z.o4[:], z.ofl[:], ALU.add, R=[z.o4, z.ofl], W=[z.o4])
            kb.tt(kb.pool, z.oa4[:], z.o4[:], z.o4[:], ALU.mult, R=[z.o4], W=[z.oa4])
            kb.op(kb.dve, lambda hh: hh.tensor_reduce(out=z.ss4[:], in_=V3(z.oa4), axis=AX.X, op=ALU.add), [z.oa4], [z.ss4])
            kb.ts(kb.dve, z.ss4[:], z.ss4[:], 1.0 / 128.0, RMS_EPS, ALU.mult, ALU.add, R=[z.ss4], W=[z.ss4])
            kb.actf(z.ss4[:], z.ss4[:], AF.Sqrt, R=[z.ss4], W=[z.ss4])
            kb.op(kb.dve, lambda hh: hh.reciprocal(out=z.ss4[:], in_=z.ss4[:]), [z.ss4], [z.ss4])
            yield
            kb.tt(kb.pool, V3(z.on4), V3(z.o4), B3(z.ss4[:]), ALU.mult, R=[z.o4, z.ss4], W=[z.on4])
            for h in range(4):
                kb.dma(kb.ld, H(z.zt, h), projT[2048 + h * 128:2048 + (h + 1) * 128, n * 128:(n + 1) * 128], W=[z.zt])
            kb.actf(z.sz[:], z.zt[:], AF.Silu, R=[z.zt], W=[z.sz])
            yield
            pot = nxtb()
            for h in range(4):
                kb.tr(H(pot, h), H(z.on4, h), identb[:], R=[z.on4, identb], W=[pot])
            kb.stt(z.gout[:], pot[:, :], gng[:, 0:1], z.sz[:], ALU.mult, ALU.mult, R=[pot, gng, z.sz], W=[z.gout])
            kb.dma(kb.st, catT[0:512, n * 128:(n + 1) * 128].rearrange("(h p) t -> p h t", p=128),
                   z.gout[:].rearrange("p (h t) -> p h t", h=4), R=[z.gout])
        yield

    class View:
        def __init__(v, pi, cs):
            v.__dict__.update(pi.__dict__)
            v.__dict__.update(cs.__dict__)

    def chain(d):
        z = SS(d)
        bt = BT()
        bss = [BS(bt), BS(bt)]
        NP = 2 if d == 1 else 1
        NCS = NP + 1
        pis = [PI() for _ in range(NP)]
        css = [CS() for _ in range(NCS)]
        kb.op(kb.dve, lambda h: h.memset(z.S32[:], 0.0), [], [z.S32])
        kb.op(kb.dve, lambda h: h.memset(z.S16[:], 0.0), [], [z.S16])
        blocks = list(range(OWN // 512)) if d == 0 else list(range(S // 512 - 1, -1, -1))
        items = []
        for bi, tb in enumerate(blocks):
            for c in (range(4) if d == 0 else range(3, -1, -1)):
                items.append((bi, tb, c))
        N = len(items)
        blk_ready = set()
        blk_gen, cur_blk, blk_next = None, None, 0
        prep_state = [None] * NP
        prep_next, prep_done = 0, set()
        scan_i, scan_gen, scans_emitted = 0, None, 0
        while scans_emitted < N:
            if blk_gen is None and blk_next < len(blocks):
                if blk_next < 2 or scans_emitted >= 4 * (blk_next - 1):
                    cur_blk = blk_next
                    blk_gen = blockprep(bss[blk_next % 2], blocks[blk_next])
                    blk_next += 1
            for p in range(NP):
                if prep_state[p] is None and prep_next < N:
                    i = prep_next
                    bi, tb, c = items[i]
                    if bi in blk_ready and (i < NCS or scans_emitted >= i - NCS + 1):
                        prep_state[p] = (i, chunkprep(View(pis[p], css[i % NCS]), bss[bi % 2], tb * 4 + c, c, d, tb < OWN // 512))
                        prep_next += 1
            if scan_gen is None and scan_i < N and scan_i in prep_done:
                bi, tb, c = items[scan_i]
                if not (d == 1 and tb < OWN // 512 and not fwd_done[0]):
                    scan_gen = scan(css[scan_i % NCS], bss[bi % 2], z, tb * 4 + c, c, d, tb < OWN // 512)
            if blk_gen is not None:
                try:
                    next(blk_gen)
                except StopIteration:
                    blk_ready.add(cur_blk)
                    blk_gen = None
            for p in range(NP):
                if prep_state[p] is not None:
                    try:
                        next(prep_state[p][1])
                    except StopIteration:
                        prep_done.add(prep_state[p][0])
                        prep_state[p] = None
            if scan_gen is not None:
                try:
                    next(scan_gen)
                except StopIteration:
                    scans_emitted += 1
                    scan_i += 1
                    scan_gen = None
            yield
        if d == 0:
            fwd_done[0] = True

    fwd_done = [False]
    for _ in _interleave([chain(0), chain(1)]):
        pass


def build_program():
    nc = bass.Bass("TRN2", target_bir_lowering=False)
    es = contextlib.ExitStack()
    dram = lambda n, shp, dt=F32, kind="ExternalInput": nc.dram_tensor(n, shp, dt, kind=kind).ap()
    x = dram("x", [S, D])
    cvec = dram("cvec", [128, 8])
    w_ada = dram("w_ada", [D, 6 * D])
    b_ada = dram("b_ada", [128, 48])
    w_in = dram("w_in", [D, 3088])
    ident_d = dram("ident", [128, 128])
    y = dram("y", [OWN, D], kind="ExternalOutput")
    projT = dram("projT", [3072, S], BF16, kind="Internal")
    abtm = dram("abtm", [128, NT * 16], F32, kind="Internal")
    modT_d = dram("modT_d", [128, 48], F32, kind="ExternalOutput" if DEBUG else "Internal")


    A = {}
    for n, shp in [("cosT", [128, S]), ("sinT", [128, S]), ("pm_d", [128, 128]), ("qg", [128, 1]), ("kg", [128, 1]),
                   ("ang", [128, 4]), ("w_out", [D, D]), ("ln1g", [128, D]), ("ln1b", [128, D]), ("ln2g", [128, D]),
                   ("ln2b", [128, D]), ("wr", [D, 36]), ("br", [128, 36]), ("w1", [32, D, 256]), ("w3", [32, D, 256]),
                   ("w2", [32, 256, D]), ("convw", [128, 60]), ("dtb", [128, 8]), ("alog", [128, 8]), ("gng", [128, 1]),
                   ("masks", [128, 12 * 128])]:
        A[n] = dram(n, shp)
    A["ident"] = ident_d
    dk = "ExternalOutput" if DEBUG else "Internal"
    catT = dram("catT", [D, OWN], BF16, kind=dk)
    x1s = dram("x1s", [OWN, D], F32, kind=dk)
    h2Ts = dram("h2Ts", [D, OWN], BF16, kind=dk)
    A["wtd"] = dram("wtd", [128, NTO * 32], F32, kind=dk)
    A["ofd"] = dram("ofd", [NTO, 128, 512], BF16, kind="Internal")
    catT_b, x1s_b, h2Ts_b = Buf(), Buf(), Buf()
    kb = KB(nc, es)
    glob = Phase(kb)
    glob.__enter__()
    ident = glob.sb("ident_s", [128, 128], F32)
    identb = glob.sb("identb", [128, 128], BF16)
    modT = glob.sb("modT", [128, 48], F32)
    sc1p = glob.sb("sc1p", [128, 8], F32)
    sc2p = glob.sb("sc2p", [128, 8], F32)
    kb.dma(kb.ld, ident[:], ident_d[:, :], W=[ident])
    kb.cp(kb.dve, identb[:], ident[:], R=[ident], W=[identb])
    projT_b = Buf()
    abtm_b = Buf()

    with Phase(kb) as ph:
        cs = ph.sb("cs", [128, 8], F32)
        bad = ph.sb("bad", [128, 48], F32)
        wa = [ph.sb(f"wa{i}", [128, 8, 512], F32) for i in range(2)]
        pm = ph.ps("pm", [128, 48], F32)
        kb.dma(kb.ld, cs[:], cvec[:, :], W=[cs])
        kb.dma(kb.ld, bad[:], b_ada[:, :], W=[bad])
        kb.actf(cs[:], cs[:], AF.Silu, R=[cs], W=[cs])
        wav = w_ada.rearrange("(k p) n -> p k n", p=128)
        for jb in range(12):
            w = wa[jb % 2]
            kb.dma(kb.ld, w[:], wav[:, :, jb * 512:(jb + 1) * 512], W=[w])
            for jj in range(4):
                j = jb * 4 + jj
                kb.mm(pm[:, j:j + 1], [(w[:, k, jj * 128:(jj + 1) * 128], cs[:, k:k + 1]) for k in range(8)],
                      R=[w, cs], W=[pm])
        kb.tt(kb.dve, modT[:], pm[:], bad[:], ALU.add, R=[pm, bad], W=[modT])
        kb.ts(kb.dve, sc1p[:], modT[:, 8:16], 1.0, None, ALU.add, R=[modT], W=[sc1p])
        kb.ts(kb.dve, sc2p[:], modT[:, 32:40], 1.0, None, ALU.add, R=[modT], W=[sc2p])
        if DEBUG:
            kb.dma(kb.st, modT_d[:, :], modT[:], R=[modT])

    if STAGE >= 1:
        with Phase(kb) as ph:
            wbf = ph.sb("wbf", [128, 8, 3088], BF16)
            wst = [ph.sb(f"wst{i}", [128, 3088], F32) for i in range(2)]
            wv = w_in.rearrange("(k p) n -> p k n", p=128)
            for k in range(8):
                w = wst[k % 2]
                kb.dma(kb.ld, w[:], wv[:, k, :], W=[w])
                kb.cp(kb.pool, wbf[:, k, :], w[:], R=[w], W=[wbf])
            xt = [ph.sb(f"xt{i}", [128, D], F32) for i in range(3)]
            xn = [ph.sb(f"xn{i}", [128, D], F32) for i in range(2)]
            hT = [ph.sb(f"hT{i}", [128, 8, 512], BF16) for i in range(2)]
            lns = LNS(ph, "b")
            ptr = [ph.ps(f"ptr{i}", [128, 512], F32) for i in range(2)]
            pmm = [ph.ps(f"pmm{i}", [128, 512], F32) for i in range(3)]
            pab = ph.ps("pab", [128, 16], F32)
            stg = [ph.sb(f"stg{i}", [128, 512], BF16) for i in range(4)]
            abs_ = ph.sb("abs", [128, NT * 16], F32)
            xv = x.rearrange("(n p) d -> n p d", p=128)
            it = 0
            ig = 0
            igc = [0]

            def ln_tile(tb, tt_):
                h_ = hT[tb % 2]
                ti = tb * 4 + tt_
                xx = xt[ti % 3]
                xo = xn[ti % 2]
                kb.dma(kb.ld, xx[:], xv[ti, :, :], W=[xx])
                stt_, mv, rstd, nmr = lns.next()
                ln_stats(kb, ph, xx, stt_, mv, rstd, nmr, "b")
                kb.actf(xo[:], xx[:], AF.Identity, R=[xx, rstd, nmr], W=[xo], scale=rstd[:, 0:1], bias=nmr[:, 0:1])
                for half in range(2):
                    p_ = ptr[half]
                    for f in range(4):
                        kb.tr(p_[:, f * 128:(f + 1) * 128], xo[:, (half * 4 + f) * 128:(half * 4 + f + 1) * 128], ident[:],
                              R=[xo, ident], W=[p_])
                    for f in range(4):
                        fc = half * 4 + f
                        kb.actf(h_[:, fc, tt_ * 128:(tt_ + 1) * 128], p_[:, f * 128:(f + 1) * 128], AF.Identity,
                                R=[p_, sc1p, modT], W=[h_], scale=sc1p[:, fc:fc + 1], bias=modT[:, fc:fc + 1])
                kb.mm(pab[:, :], [(h_[:, k, tt_ * 128:(tt_ + 1) * 128], wbf[:, k, 3072:3088]) for k in range(8)],
                      R=[h_, wbf], W=[pab])
                kb.cp(kb.dve, abs_[:, ti * 16:(ti + 1) * 16], pab[:, :], R=[pab], W=[abs_])

            def mm_group(tb, cg):
                h_ = hT[tb % 2]
                ig = igc[0]
                igc[0] += 1
                p_ = pmm[ig % 3]
                s_ = stg[ig % 4]
                kb.mm(p_[:, :], [(wbf[:, k, cg * 128:(cg + 1) * 128], h_[:, k, :]) for k in range(8)],
                      R=[wbf, h_], W=[p_])
                if ig % 2 == 0:
                    kb.cp(kb.dve, s_[:], p_[:, :], R=[p_], W=[s_])
                else:
                    kb.cp(kb.act, s_[:], p_[:, :], R=[p_], W=[s_])
                kb.dma(kb.st, projT[cg * 128:(cg + 1) * 128, tb * 512:(tb + 1) * 512], s_[:], R=[s_])

            NB = S // 512
            for tt_ in range(4):
                ln_tile(0, tt_)
            for tb in range(NB):
                ngrp = 24 if tb < OWN // 512 else (16 if tb == OWN // 512 else 12)
                per = ngrp // 4
                for j in range(4):
                    for cg in range(j * per, (j + 1) * per):
                        mm_group(tb, cg)
                    if tb + 1 < NB:
                        ln_tile(tb + 1, j)
            kb.dma(kb.st, abtm[:, :], abs_[:], R=[abs_])


    if STAGE >= 2:
        with Phase(kb) as ph:
            attention_phase(kb, ph, nc, A, ident, identb, projT, projT_b, catT, catT_b)
    if STAGE >= 3 and not SKIP_GDN:
        with Phase(kb) as ph:
            gdn_phase(kb, ph, nc, A, ident, identb, projT, projT_b, abtm, abtm_b, catT, catT_b)
    elif STAGE >= 3:
        with Phase(kb) as ph:
            zt = ph.sb("zt", [128, 4096], BF16)
            kb.op(kb.dve, lambda h: h.memset(zt[:], 0.0), [], [zt])
            for r in range(4):
                kb.dma(kb.st, catT[r * 128:(r + 1) * 128, :], zt[:], R=[zt])
    if STAGE >= 4:
        wt_all = glob.sb("wt_all", [128, NTO, 32], F32)
        gt2rep = glob.sb("gt2rep", [128, D], F32)
        with Phase(kb) as ph:
            wout_phase(kb, ph, nc, A, ident, identb, modT, sc2p, catT, catT_b, x, x1s, x1s_b, h2Ts, h2Ts_b, wt_all, gt2rep)
    if STAGE >= 5:
        with Phase(kb) as ph:
            moe_phase(kb, ph, nc, A, x1s, x1s_b, h2Ts, h2Ts_b, wt_all, gt2rep, y)
    if STAGE < 99:
        with Phase(kb) as ph:
            z = ph.sb("z", [128, D], F32)
            kb.op(kb.dve, lambda h: h.memset(z[:], 0.0), [], [z])
            kb.dma(kb.st, y[0:128, :], z[:], R=[z])
    kb.final_wait()
    glob.__exit__(None, None, None)
    es.close()
    return nc


def _rope_tables(pos_tok):
    inv = 10000.0 ** (-np.arange(0, 64, 2, dtype=np.float32) / 64.0)
    row = (pos_tok // 64).astype(np.float32)
    col = (pos_tok % 64).astype(np.float32)
    ang = np.zeros((128, pos_tok.shape[0]), np.float32)
    for d in range(128):
        p = row if d < 64 else col
        ang[d] = p * inv[d % 32]
    return np.cos(ang).astype(np.float32), np.sin(ang).astype(np.float32)


def _gdn_masks():
    i = np.arange(128)
    bd = lambda s: ((i[:, None] // s) == (i[None, :] // s)).astype(np.float32)
    sl = (i[:, None] > i[None, :]).astype(np.float32)
    out = []
    for d in range(2):
        s_ = sl if d == 0 else sl.T
        cu = (i[:, None] <= i[None, :]).astype(np.float32) if d == 0 else (i[:, None] >= i[None, :]).astype(np.float32)
        out += [s_, cu, -bd(16) * s_, (bd(32) - bd(16)) * s_, (bd(64) - bd(32)) * s_, (1.0 - bd(64)) * s_]
    return np.concatenate(out, axis=1).astype(np.float32)


def make_in_maps(inputs):
    maps = []
    ident = np.eye(128, dtype=np.float32)
    f = lambda k: np.asarray(inputs[k], np.float32)
    x = f("x")
    c = f("c")
    w_in = f("w_in")[0]
    gq, gk, gv, gz = w_in[:, 0:512], w_in[:, 512:1024], w_in[:, 1024:1536], w_in[:, 1536:2048]
    ab = w_in[:, 2048:2064]
    aq, ak, av = w_in[:, 2064:2576], w_in[:, 2576:2832], w_in[:, 2832:3088]
    pm = np.zeros((128, 128), np.float32)
    for d in range(128):
        if (d % 64) < 32:
            pm[d + 32, d] = -1.0
        else:
            pm[d - 32, d] = 1.0
    rep = lambda v: np.ascontiguousarray(np.broadcast_to(np.asarray(v, np.float32).reshape(1, -1), (128, np.asarray(v).size)))
    conv = f("conv_w")[0]
    a_log = f("a_log")[0]
    dt_bias = f("dt_bias")[0]
    masks = _gdn_masks()
    shared = {
        "w_ada": np.ascontiguousarray(f("w_ada")[0]),
        "b_ada": np.ascontiguousarray(f("b_ada")[0].reshape(48, 128).T),
        "ident": ident, "pm_d": pm,
        "qg": np.ascontiguousarray(f("q_norm_g")[0].reshape(128, 1)),
        "kg": np.ascontiguousarray(f("k_norm_g")[0].reshape(128, 1)),
        "ang": np.ascontiguousarray(f("attn_norm_g")[0].reshape(4, 128).T),
        "w_out": np.ascontiguousarray(f("w_out")[0]),
        "ln1g": rep(f("ln1_g")[0]), "ln1b": rep(f("ln1_b")[0]), "ln2g": rep(f("ln2_g")[0]), "ln2b": rep(f("ln2_b")[0]),
        "wr": np.ascontiguousarray(np.concatenate([f("w_group")[0], f("w_router")[0]], axis=1)),
        "br": rep(np.concatenate([f("b_group")[0], f("b_router")[0]])),
        "w1": np.ascontiguousarray(f("w1")[0]), "w3": np.ascontiguousarray(f("w3")[0]), "w2": np.ascontiguousarray(f("w2")[0]),
        "gng": np.ascontiguousarray(f("gdn_norm_g")[0].reshape(128, 1)),
        "masks": masks,
    }
    for core in range(8):
        b, half = core // 2, core % 2
        pos = np.arange(S) if half == 0 else np.arange(S)[::-1]
        xl = x[b] if half == 0 else x[b][::-1]
        a_f, a_b, b_f, b_b = ab[:, 0:4], ab[:, 4:8], ab[:, 8:12], ab[:, 12:16]
        if half == 0:
            abl = np.concatenate([a_f, a_b, b_f, b_b], axis=1)
            dirs = [0, 1]
            cw = conv
        else:
            abl = np.concatenate([a_b, a_f, b_b, b_f], axis=1)
            dirs = [1, 0]
            cw = conv[::-1]
        w_in_p = np.concatenate([gk, gv, ak, av, gq, gz, aq, abl], axis=1)
        cwp = np.concatenate([cw[:, 512:1024], cw[:, 1024:1536], cw[:, 0:512]], axis=1)
        convw = np.ascontiguousarray(cwp.T.reshape(12, 128, 5).transpose(1, 0, 2).reshape(128, 60))
        cosT, sinT = _rope_tables(pos)
        m = dict(shared)
        m.update({
            "x": np.ascontiguousarray(xl),
            "cvec": np.ascontiguousarray(c[b].reshape(8, 128).T),
            "w_in": np.ascontiguousarray(w_in_p),
            "cosT": cosT, "sinT": sinT,
            "convw": convw,
            "dtb": rep(np.concatenate([dt_bias[dirs[0]], dt_bias[dirs[1]]])),
            "alog": rep(np.concatenate([a_log[dirs[0]], a_log[dirs[1]]])),
        })
        maps.append(m)
    return maps


def kernel(**inputs):
    nc = build_program()
    maps = make_in_maps(inputs)
    res = run_bass_kernel_spmd(nc, maps, core_ids=list(range(8)))
    out = np.zeros((4, S, D), np.float32)
    for core in range(8):
        b, half = core // 2, core % 2
        yl = np.asarray(res.results[core]["y"], np.float32)
        if half == 0:
            out[b, :OWN] = yl
        else:
            out[b, OWN:] = yl[::-1]
    kernel.last = res
    return out
```

```python
import contextlib
import numpy as np
import concourse.bass as bass
import concourse.mybir as mybir
from concourse.bass_utils import run_bass_kernel_spmd

F32 = mybir.dt.float32
BF16 = mybir.dt.bfloat16
AF = mybir.ActivationFunctionType
ALU = mybir.AluOpType
AX = mybir.AxisListType

D = 1024
S = 8192
OWN = 4096
NT = S // 128
NTO = OWN // 128
LN_EPS = 1e-5
RMS_EPS = 1e-6
ALPHA = 2.0 ** 0.25
STAGE = 99
SKIP_GDN = False
SAME_ENGINE_WAIT = True
DEBUG = False


class Buf:
    __slots__ = ("w", "r", "excl")

    def __init__(self):
        self.w = None
        self.r = {}
        self.excl = False


class Stream:
    def __init__(self, h):
        self.h = h
        self.seen = {}


class Eng:
    def __init__(self, name, stream, sems, dma=False):
        self.name = name
        self.stream = stream
        self.sems = sems
        self.dma = dma
        self.count = 0
        self.uses = [0] * len(sems)
        self.rr = 0


class TL:
    def __init__(self, t):
        self.t = t
        self.b = Buf()

    def __getitem__(self, k):
        return self.t[k]


class KB:
    def __init__(self, nc, es):
        self.nc = nc
        self.es = es
        mk = lambda n: es.enter_context(nc.semaphore(n))
        self.s_pe = Stream(nc.tensor)
        self.s_act = Stream(nc.scalar)
        self.s_dve = Stream(nc.vector)
        self.s_pool = Stream(nc.gpsimd)
        self.s_sp = Stream(nc.sync)
        self.pe = Eng("pe", self.s_pe, [mk("s_pe")])
        self.act = Eng("act", self.s_act, [mk("s_act")])
        self.dve = Eng("dve", self.s_dve, [mk("s_dve")])
        self.pool = Eng("pool", self.s_pool, [mk("s_pool")])
        self.ld = Eng("ld", self.s_sp, [mk(f"s_ld{i}") for i in range(8)], dma=True)
        self.st = Eng("st", self.s_pool, [mk(f"s_st{i}") for i in range(6)], dma=True)
        self.n_inst = 0

    def _wait(self, st, tok, eng=None):
        if tok is None:
            return
        sem, val = tok
        key = id(sem)
        if st.seen.get(key, 0) >= val:
            return
        if eng is not None and (not SAME_ENGINE_WAIT or eng.name == "pe") and not eng.dma and sem is eng.sems[0]:
            return
        st.h.wait_ge(sem, val)
        st.seen[key] = val

    def op(self, eng, fn, R=(), W=(), sig=True):
        st = eng.stream
        R = [x.b if isinstance(x, TL) else x for x in R]
        W = [x.b if isinstance(x, TL) else x for x in W]
        W = W + [b for b in R if b.excl and b not in W]
        R = [b for b in R if not b.excl]
        need = {}

        def add(t):
            if t is None:
                return
            k = id(t[0])
            if k not in need or need[k][1] < t[1]:
                need[k] = t

        for b in R:
            add(b.w)
        for b in W:
            add(b.w)
            for t in b.r.values():
                add(t)
        for t in need.values():
            self._wait(st, t, eng)
        if eng.dma:
            i = eng.rr
            eng.rr = (eng.rr + 1) % len(eng.sems)
            sem = eng.sems[i]
            if eng.uses[i] > 0:
                self._wait(st, (sem, 16 * eng.uses[i]))
            inst = fn(st.h)
            eng.uses[i] += 1
            inst.then_inc(sem, 16)
            tok = (sem, 16 * eng.uses[i])
        else:
            inst = fn(st.h)
            sem = eng.sems[0]
            tok = (sem, eng.count + 1)
            if sig:
                eng.count += 1
                inst.then_inc(sem, 1)
        self.n_inst += 1
        for b in R:
            k = id(tok[0])
            if k not in b.r or b.r[k][1] < tok[1]:
                b.r[k] = tok
        for b in W:
            b.w = tok
            b.r = {}
        return tok

    def dma(self, q, out, in_, R=(), W=()):
        return self.op(q, lambda h: h.dma_start(out=out, in_=in_), R, W)

    def mm(self, out, pairs, R=(), W=(), transpose=False):
        n = len(pairs)
        for i, (l, r) in enumerate(pairs):
            self.op(self.pe,
                    lambda h, l=l, r=r, i=i: h.matmul(out, l, r, start=(i == 0), stop=(i == n - 1)),
                    R, W, sig=(i == n - 1))

    def tr(self, out, in_, ident, R=(), W=()):
        self.op(self.pe, lambda h: h.transpose(out, in_, ident), R, W)

    def actf(self, out, in_, func, R=(), W=(), **kw):
        self.op(self.act, lambda h: h.activation(out=out, in_=in_, func=func, **kw), R, W)

    def ts(self, eng, out, in0, s1, s2, op0, op1=None, R=(), W=(), **kw):
        if op1 is None:
            self.op(eng, lambda h: h.tensor_scalar(out=out, in0=in0, scalar1=s1, scalar2=None, op0=op0, **kw), R, W)
        else:
            self.op(eng, lambda h: h.tensor_scalar(out=out, in0=in0, scalar1=s1, scalar2=s2, op0=op0, op1=op1, **kw), R, W)

    def tt(self, eng, out, in0, in1, op, R=(), W=()):
        self.op(eng, lambda h: h.tensor_tensor(out=out, in0=in0, in1=in1, op=op), R, W)

    def stt(self, out, in0, scalar, in1, op0, op1, R=(), W=()):
        self.op(self.dve, lambda h: h.scalar_tensor_tensor(out=out, in0=in0, scalar=scalar, in1=in1, op0=op0, op1=op1), R, W)

    def cp(self, eng, out, in_, R=(), W=()):
        if eng is self.act:
            self.op(eng, lambda h: h.copy(out=out, in_=in_), R, W)
        else:
            self.op(eng, lambda h: h.tensor_copy(out=out, in_=in_), R, W)

    def barrier(self):
        toks = []
        for e in (self.pe, self.act, self.dve, self.pool):
            if e.count > 0:
                toks.append((e.sems[0], e.count))
        for q in (self.st, self.ld):
            for i, sem in enumerate(q.sems):
                if q.uses[i] > 0:
                    toks.append((sem, 16 * q.uses[i]))
        for st in (self.s_pe, self.s_act, self.s_dve, self.s_pool, self.s_sp):
            for t in toks:
                self._wait(st, t)

    def final_wait(self):
        for q in (self.st, self.ld):
            for i, sem in enumerate(q.sems):
                if q.uses[i] > 0:
                    self._wait(self.s_sp, (sem, 16 * q.uses[i]))


class Phase:
    def __init__(self, kb):
        self.kb = kb
        self.es = contextlib.ExitStack()
        self.n = 0

    def __enter__(self):
        self.es.__enter__()
        return self

    def __exit__(self, *a):
        self.kb.barrier()
        return self.es.__exit__(*a)

    def sb(self, name, shape, dt):
        KB.uid = getattr(KB, "uid", 0) + 1
        return TL(self.es.enter_context(self.kb.nc.sbuf_tensor(f"sb{KB.uid}_{name}", shape, dt)))

    def ps(self, name, shape, dt):
        KB.uid = getattr(KB, "uid", 0) + 1
        t = TL(self.es.enter_context(self.kb.nc.psum_tensor(f"ps{KB.uid}_{name}", shape, dt)))
        t.b.excl = True
        return t


class LNS:
    def __init__(self, ph, tag, n=3):
        n = max(n, 3)
        self.sets = [tuple(ph.sb(f"{tag}{nm}{i}", shp, F32) for nm, shp in (("stt", [128, 12]), ("mv", [128, 2]), ("rstd", [128, 1]), ("nmr", [128, 1])))
                     for i in range(n)]
        self.i = 0

    def next(self):
        self.i += 1
        return self.sets[self.i % len(self.sets)]


def ln_stats(kb, ph, xt, stt_, mv, rstd, nmr, tag):
    for c in range(2):
        kb.op(kb.dve, lambda h, c=c: h.bn_stats(out=stt_[:, c * 6:(c + 1) * 6], in_=xt[:, c * 512:(c + 1) * 512]), [xt], [stt_])
    kb.op(kb.dve, lambda h: h.bn_aggr(out=mv[:, 0:2], in_=stt_[:, 0:12]), [stt_], [mv])
    kb.ts(kb.dve, rstd[:, 0:1], mv[:, 1:2], LN_EPS, None, ALU.add, R=[mv], W=[rstd])
    kb.actf(rstd[:, 0:1], rstd[:, 0:1], AF.Sqrt, R=[rstd], W=[rstd])
    kb.op(kb.dve, lambda h: h.reciprocal(out=rstd[:, 0:1], in_=rstd[:, 0:1]), [rstd], [rstd])
    kb.stt(nmr[:, 0:1], mv[:, 0:1], -1.0, rstd[:, 0:1], ALU.mult, ALU.mult, R=[mv, rstd], W=[nmr])


def const_col(kb, ph, name, val):
    t = ph.sb(name, [128, 1], F32)
    kb.op(kb.dve, lambda h: h.memset(t[:], float(val)), [], [t])
    return t


def attention_phase(kb, ph, nc, A, ident, identb, projT, projT_b, catT, catT_b):
    SC = 128.0 ** -0.5
    KT = ph.sb("KT", [128, 2, S], BF16)
    VT = ph.sb("VT", [128, NT, 2, 128], BF16)
    QT = ph.sb("QT", [128, 4, OWN], BF16)
    onesb = ph.sb("onesb", [128, 128], BF16)
    pmf = ph.sb("pmf", [128, 128], F32)
    pmb = ph.sb("pmb", [128, 128], BF16)
    qg = ph.sb("qg", [128, 1], F32)
    kg = ph.sb("kg", [128, 1], F32)
    ang = ph.sb("ang", [128, 4], F32)
    eps1 = const_col(kb, ph, "eps1", RMS_EPS)
    kb.op(kb.dve, lambda h: h.memset(onesb[:], 1.0), [], [onesb])
    kb.dma(kb.ld, pmf[:], A["pm_d"][:, :], W=[pmf])
    kb.cp(kb.dve, pmb[:], pmf[:], R=[pmf], W=[pmb])
    kb.dma(kb.ld, qg[:], A["qg"][:, :], W=[qg])
    kb.dma(kb.ld, kg[:], A["kg"][:, :], W=[kg])
    kb.dma(kb.ld, ang[:], A["ang"][:, :], W=[ang])
    xin = [ph.sb(f"axin{i}", [128, 512], BF16) for i in range(3)]
    cs_ = [ph.sb(f"acs{i}", [128, 512], F32) for i in range(2)]
    sn_ = [ph.sb(f"asn{i}", [128, 512], F32) for i in range(2)]
    sq = ph.sb("asq", [128, 512], BF16)
    rs = ph.sb("ars", [128, 512], F32)
    xnb = ph.sb("axnb", [128, 512], BF16)
    t1 = ph.sb("at1", [128, 512], F32)
    t2 = ph.sb("at2", [128, 512], F32)
    pS2 = [ph.ps(f"apS2{i}", [128, 1024], F32) for i in range(2)]
    pO = [ph.ps(f"apO{i}", [128, 512], F32) for i in range(2)]
    pL = [ph.ps(f"apL{i}", [128, 512], F32) for i in range(1)]
    pVt = ph.ps("apV", [128, 512], BF16)
    li = 0
    for tb in range(S // 512):
        cc, ss_ = cs_[tb % 2], sn_[tb % 2]
        kb.dma(kb.ld, cc[:], A["cosT"][:, tb * 512:(tb + 1) * 512], W=[cc])
        kb.dma(kb.ld, ss_[:], A["sinT"][:, tb * 512:(tb + 1) * 512], W=[ss_])
        jobs = [("k", 0), ("k", 1)]
        if tb < OWN // 512:
            jobs += [("q", i) for i in range(4)]
        for kind, hh in jobs:
            xi = xin[li % 3]
            li += 1
            row0 = (1024 + hh * 128) if kind == "k" else (2560 + hh * 128)
            kb.dma(kb.ld, xi[:], projT[row0:row0 + 128, tb * 512:(tb + 1) * 512], R=[projT_b], W=[xi])
            kb.actf(sq[:], xi[:], AF.Square, R=[xi], W=[sq])
            p_ = pS2[0]
            kb.mm(p_[:, 0:512], [(onesb[:], sq[:])], R=[onesb, sq], W=[p_])
            kb.actf(rs[:], p_[:, 0:512], AF.Sqrt, R=[p_, eps1], W=[rs], scale=1.0 / 128.0, bias=eps1[:, 0:1])
            kb.op(kb.dve, lambda h: h.reciprocal(out=rs[:], in_=rs[:]), [rs], [rs])
            g_ = kg if kind == "k" else qg
            kb.stt(xnb[:], xi[:], g_[:, 0:1], rs[:], ALU.mult, ALU.mult, R=[xi, g_, rs], W=[xnb])
            p2 = pS2[1]
            kb.mm(p2[:, 0:512], [(pmb[:], xnb[:])], R=[pmb, xnb], W=[p2])
            kb.tt(kb.dve, t1[:], xnb[:], cc[:], ALU.mult, R=[xnb, cc], W=[t1])
            kb.tt(kb.dve, t2[:], p2[:, 0:512], ss_[:], ALU.mult, R=[p2, ss_], W=[t2])
            if kind == "k":
                kb.tt(kb.pool, KT[:, hh, tb * 512:(tb + 1) * 512], t1[:], t2[:], ALU.add, R=[t1, t2], W=[KT])
            else:
                kb.tt(kb.pool, QT[:, hh, tb * 512:(tb + 1) * 512], t1[:], t2[:], ALU.add, R=[t1, t2], W=[QT])
        for hh in range(2):
            xi = xin[li % 3]
            li += 1
            row0 = 1280 + hh * 128
            kb.dma(kb.ld, xi[:], projT[row0:row0 + 128, tb * 512:(tb + 1) * 512], R=[projT_b], W=[xi])
            for f in range(4):
                kb.tr(pVt[:, f * 128:(f + 1) * 128], xi[:, f * 128:(f + 1) * 128], identb[:], R=[xi, identb], W=[pVt])
            for f in range(4):
                kb.cp(kb.act, VT[:, tb * 4 + f, hh, :], pVt[:, f * 128:(f + 1) * 128], R=[pVt], W=[VT])
    pt2 = [ph.sb(f"apt2{i}", [128, 1024], BF16) for i in range(3)]
    araw = ph.sb("araw", [128, 4, 512], F32)
    accD = ph.sb("aaccD", [128, 512], F32)
    accP = ph.sb("aaccP", [128, 512], F32)
    accD1 = ph.sb("aaccD1", [128, 512], F32)
    ones32a = ph.sb("aones32", [128, 128], F32)
    kb.op(kb.dve, lambda h: h.memset(ones32a[:], 1.0), [], [ones32a])
    rl = ph.sb("arl", [128, 512], F32)
    aout = [ph.sb(f"aout{i}", [128, 512], BF16) for i in range(2)]
    gi = 0
    ao = 0
    for qb in range(OWN // 512):
        for qh in range(4):
            kvh = qh // 2
            po, pl = pO[gi % 2], pL[0]
            gi += 1
            qsl = QT[:, qh, qb * 512:(qb + 1) * 512]

            def s_mm2(kp):
                p_ = pS2[kp % 2]
                for j in range(2):
                    kt = 2 * kp + j
                    kb.mm(p_[:, j * 512:(j + 1) * 512], [(KT[:, kvh, kt * 128:(kt + 1) * 128], qsl)], R=[KT, QT], W=[p_])

            def e_pv2(kp):
                p_ = pS2[kp % 2]
                e_ = pt2[kp % 3]
                kb.actf(e_[:], p_[:, :], AF.Exp, R=[p_], W=[e_], scale=SC)
                for j in range(2):
                    kt = 2 * kp + j
                    kb.op(kb.pe, lambda h, kt=kt, j=j: h.matmul(po[:, :], VT[:, kt, kvh, :], e_[:, j * 512:(j + 1) * 512],
                                                                start=(kt == 0), stop=(kt == NT - 1)), [VT, e_], [po])
                h0, h1 = e_[:, 0:512], e_[:, 512:1024]
                ad = accD if kp % 2 == 0 else accD1
                if kp < 2:
                    kb.cp(kb.dve, ad[:], h0, R=[e_], W=[ad])
                else:
                    kb.tt(kb.dve, ad[:], ad[:], h0, ALU.add, R=[ad, e_], W=[ad])
                if kp % 2 == 0:
                    kb.op(kb.pe, lambda h: h.matmul(pl[:, :], onesb[:], h1, start=(kp == 0), stop=False), [onesb, e_], [pl])
                elif kp == 1:
                    kb.cp(kb.pool, accP[:], h1, R=[e_], W=[accP])
                else:
                    kb.tt(kb.pool, accP[:], accP[:], h1, ALU.add, R=[accP, e_], W=[accP])

            s_mm2(0)
            for kp in range(NT // 2):
                if kp + 1 < NT // 2:
                    s_mm2(kp + 1)
                e_pv2(kp)
            for ai, a_ in enumerate((accD, accD1, accP)):
                kb.op(kb.pe, lambda h, a_=a_, ai=ai: h.matmul(pl[:, :], ones32a[:], a_[:], start=False, stop=(ai == 2)),
                      [ones32a, a_], [pl])
            kb.op(kb.dve, lambda h: h.reciprocal(out=rl[:], in_=pl[:, :]), [pl], [rl])
            kb.tt(kb.dve, araw[:, qh, :], po[:, :], rl[:], ALU.mult, R=[po, rl], W=[araw])
        pn = pO[0]
        for qh in range(4):
            kb.actf(sq[:], araw[:, qh, :], AF.Square, R=[araw], W=[sq])
            kb.op(kb.pe, lambda h, qh=qh: h.matmul(pn[:, :], onesb[:], sq[:], start=(qh == 0), stop=(qh == 3)), [onesb, sq], [pn])
        kb.actf(rs[:], pn[:, :], AF.Sqrt, R=[pn, eps1], W=[rs], scale=1.0 / 512.0, bias=eps1[:, 0:1])
        kb.op(kb.dve, lambda h: h.reciprocal(out=rs[:], in_=rs[:]), [rs], [rs])
        for qh in range(4):
            o_ = aout[ao % 2]
            ao += 1
            kb.stt(o_[:], araw[:, qh, :], ang[:, qh:qh + 1], rs[:], ALU.mult, ALU.mult, R=[araw, ang, rs], W=[o_])
            kb.dma(kb.st, catT[512 + qh * 128:512 + (qh + 1) * 128, qb * 512:(qb + 1) * 512], o_[:], R=[o_])


def rep_from_cols(kb, ph, ident, cols, dst, pX, dg, ones32):
    for c in range(8):
        kb.ts(kb.dve, dg[:], ident[:], cols[:, c:c + 1], None, ALU.mult, R=[ident, cols], W=[dg])
        kb.mm(pX[:, (c % 4) * 128:(c % 4 + 1) * 128], [(ones32[:], dg[:])], R=[ones32, dg], W=[pX])
        if c % 4 == 3:
            kb.cp(kb.dve, dst[:, (c - 3) * 128:(c + 1) * 128], pX[:, :], R=[pX], W=[dst])


def wout_phase(kb, ph, nc, A, ident, identb, modT, sc2p, catT, catT_b, x, x1s, x1s_b, h2Ts, h2Ts_b, wt_all, gt2rep):
    wob = ph.sb("wob", [128, 8, D], BF16)
    wst = [ph.sb(f"wost{i}", [128, D], F32) for i in range(2)]
    wv = A["w_out"].rearrange("(k p) n -> p k n", p=128)
    for k in range(8):
        w = wst[k % 2]
        kb.dma(kb.ld, w[:], wv[:, k, :], W=[w])
        kb.cp(kb.pool, wob[:, k, :], w[:], R=[w], W=[wob])
    ones32 = ph.sb("ones32", [128, 128], F32)
    kb.op(kb.dve, lambda h: h.memset(ones32[:], 1.0), [], [ones32])
    dg = ph.sb("dg", [128, 128], F32)
    gt1rep = ph.sb("gt1rep", [128, D], F32)
    pX = ph.ps("pX", [128, 512], F32)
    gt1c = ph.sb("gt1c", [128, 8], F32)
    gt2c = ph.sb("gt2c", [128, 8], F32)
    kb.cp(kb.dve, gt1c[:], modT[:, 16:24], R=[modT], W=[gt1c])
    kb.cp(kb.dve, gt2c[:], modT[:, 40:48], R=[modT], W=[gt2c])
    rep_from_cols(kb, ph, ident, gt1c, gt1rep, pX, dg, ones32)
    rep_from_cols(kb, ph, ident, gt2c, gt2rep, pX, dg, ones32)
    g1 = ph.sb("ln1g", [128, D], F32)
    b1 = ph.sb("ln1b", [128, D], F32)
    kb.dma(kb.ld, g1[:], A["ln1g"][:, :], W=[g1])
    kb.dma(kb.ld, b1[:], A["ln1b"][:, :], W=[b1])
    wr32 = ph.sb("wr32", [128, 8, 36], F32)
    kb.dma(kb.ld, wr32[:], A["wr"].rearrange("(k p) n -> p k n", p=128), W=[wr32])
    br = ph.sb("br", [128, 36], F32)
    kb.dma(kb.ld, br[:], A["br"][:, :], W=[br])
    lns = LNS(ph, "w", 6)
    pM = [ph.ps(f"wpM{i}", [128, 512], F32) for i in range(2)]
    pT_ = [ph.ps(f"wpT{i}", [128, 512], F32) for i in range(2)]
    pR = ph.ps("wpR", [128, 36], F32)

    class WS:
        def __init__(w, i):
            w.cat = ph.sb(f"cat{i}", [128, 8, 128], BF16)
            w.xt = ph.sb(f"wxt{i}", [128, D], F32)
            w.tmp = ph.sb(f"wtmp{i}", [128, D], F32)
            w.x1 = ph.sb(f"wx1{i}", [128, D], F32)
            w.xn2 = ph.sb(f"wxn2{i}", [128, D], F32)
            w.h2f = ph.sb(f"h2f{i}", [128, 8, 128], F32)
            w.h2b = ph.sb(f"h2b{i}", [128, 8, 128], BF16)
            w.lg = ph.sb(f"lg{i}", [128, 36], F32)
            w.sm = ph.sb(f"rsm{i}", [128, 16], F32)
            w.ohg, w.mb, w.eg = [ph.sb(f"r4{j}_{i}", [128, 4], F32) for j in range(3)]
            w.ml, w.ml2, w.oh1, w.oh2 = [ph.sb(f"r32{j}_{i}", [128, 32], F32) for j in range(4)]

    catv = catT.rearrange("(k p) t -> p k t", p=128)
    h2v = h2Ts.rearrange("(k p) t -> p k t", p=128)
    xv = x.rearrange("(n p) d -> n p d", p=128)
    x1v = x1s.rearrange("(n p) d -> n p d", p=128)

    def tile_gen(ti, w):
        c_, xx, tmp, xo, xn2, h2f, hb = w.cat, w.xt, w.tmp, w.x1, w.xn2, w.h2f, w.h2b
        lg, sm, ohg, mb, eg, ml, ml2, oh1, oh2 = w.lg, w.sm, w.ohg, w.mb, w.eg, w.ml, w.ml2, w.oh1, w.oh2
        kb.dma(kb.ld, c_[:], catv[:, :, ti * 128:(ti + 1) * 128], W=[c_])
        kb.dma(kb.ld, xx[:], xv[ti, :, :], W=[xx])
        yield
        for nb in range(2):
            kb.mm(pM[nb][:, :], [(c_[:, k, :], wob[:, k, nb * 512:(nb + 1) * 512]) for k in range(8)], R=[c_, wob], W=[pM[nb]])
            kb.tt(kb.dve, tmp[:, nb * 512:(nb + 1) * 512], pM[nb][:, :], gt1rep[:, nb * 512:(nb + 1) * 512], ALU.mult,
                  R=[pM[nb], gt1rep], W=[tmp])
        kb.stt(tmp[:], xx[:], ALPHA, tmp[:], ALU.mult, ALU.add, R=[xx, tmp], W=[tmp])
        yield
        stt_, mv, rstd, nmr = lns.next()
        ln_stats(kb, ph, tmp, stt_, mv, rstd, nmr, "e1")
        yield
        kb.actf(xo[:], tmp[:], AF.Identity, R=[tmp, rstd, nmr], W=[xo], scale=rstd[:, 0:1], bias=nmr[:, 0:1])
        kb.tt(kb.pool, xo[:], xo[:], g1[:], ALU.mult, R=[xo, g1], W=[xo])
        kb.tt(kb.pool, xo[:], xo[:], b1[:], ALU.add, R=[xo, b1], W=[xo])
        kb.dma(kb.st, x1v[ti, :, :], xo[:], R=[xo])
        yield
        stt_, mv, rstd, nmr = lns.next()
        ln_stats(kb, ph, xo, stt_, mv, rstd, nmr, "e2")
        yield
        kb.actf(xn2[:], xo[:], AF.Identity, R=[xo, rstd, nmr], W=[xn2], scale=rstd[:, 0:1], bias=nmr[:, 0:1])
        yield
        for half in range(2):
            p_ = pT_[half]
            for f in range(4):
                kb.tr(p_[:, f * 128:(f + 1) * 128], xn2[:, (half * 4 + f) * 128:(half * 4 + f + 1) * 128], ident[:],
                      R=[xn2, ident], W=[p_])
            for f in range(4):
                fc = half * 4 + f
                kb.actf(h2f[:, fc, :], p_[:, f * 128:(f + 1) * 128], AF.Identity,
                        R=[p_, sc2p, modT], W=[h2f], scale=sc2p[:, fc:fc + 1], bias=modT[:, 24 + fc:25 + fc])
            yield
        kb.cp(kb.pool, hb[:], h2f[:], R=[h2f], W=[hb])
        kb.dma(kb.st, h2v[:, :, ti * 128:(ti + 1) * 128], hb[:], R=[hb])
        kb.mm(pR[:, :], [(h2f[:, k, :], wr32[:, k, :]) for k in range(8)], R=[h2f, wr32], W=[pR])
        kb.tt(kb.dve, lg[:], pR[:, :], br[:], ALU.add, R=[pR, br], W=[lg])
        yield
        dv = kb.dve
        kb.op(dv, lambda h: h.tensor_reduce(out=sm[:, 0:1], in_=lg[:, 0:4], axis=AX.X, op=ALU.max), [lg], [sm])
        kb.ts(dv, ohg[:], lg[:, 0:4], sm[:, 0:1], None, ALU.is_equal, R=[lg, sm], W=[ohg])
        kb.ts(dv, sm[:, 1:2], sm[:, 0:1], -1.0, None, ALU.mult, R=[sm], W=[sm])
        yield
        kb.actf(eg[:], lg[:, 0:4], AF.Exp, R=[lg, sm], W=[eg, sm], bias=sm[:, 1:2], accum_out=sm[:, 2:3])
        kb.op(dv, lambda h: h.reciprocal(out=sm[:, 3:4], in_=sm[:, 2:3]), [sm], [sm])
        kb.ts(dv, mb[:], ohg[:], -1.0, 1e30, ALU.add, ALU.mult, R=[ohg], W=[mb])
        yield
        for g in range(4):
            kb.ts(dv, ml[:, g * 8:(g + 1) * 8], lg[:, 4 + g * 8:12 + g * 8], mb[:, g:g + 1], None, ALU.add, R=[lg, mb], W=[ml])
        yield
        kb.op(dv, lambda h: h.tensor_reduce(out=sm[:, 4:5], in_=ml[:], axis=AX.X, op=ALU.max), [ml], [sm])
        kb.ts(dv, oh1[:], ml[:], sm[:, 4:5], None, ALU.is_equal, R=[ml, sm], W=[oh1])
        yield
        kb.stt(ml2[:], oh1[:], -1e30, ml[:], ALU.mult, ALU.add, R=[oh1, ml], W=[ml2])
        kb.op(dv, lambda h: h.tensor_reduce(out=sm[:, 5:6], in_=ml2[:], axis=AX.X, op=ALU.max), [ml2], [sm])
        yield
        kb.ts(dv, oh2[:], ml2[:], sm[:, 5:6], None, ALU.is_equal, R=[ml2, sm], W=[oh2])
        kb.ts(dv, sm[:, 6:7], sm[:, 4:5], -1.0, None, ALU.mult, R=[sm], W=[sm])
        yield
        kb.actf(sm[:, 7:8], sm[:, 5:6], AF.Exp, R=[sm], W=[sm], bias=sm[:, 6:7])
        kb.ts(dv, sm[:, 8:9], sm[:, 7:8], 1.0, None, ALU.add, R=[sm], W=[sm])
        yield
        kb.op(dv, lambda h: h.reciprocal(out=sm[:, 9:10], in_=sm[:, 8:9]), [sm], [sm])
        kb.tt(dv, sm[:, 10:11], sm[:, 9:10], sm[:, 3:4], ALU.mult, R=[sm], W=[sm])
        yield
        kb.tt(dv, sm[:, 11:12], sm[:, 10:11], sm[:, 7:8], ALU.mult, R=[sm], W=[sm])
        kb.ts(dv, oh1[:], oh1[:], sm[:, 10:11], None, ALU.mult, R=[oh1, sm], W=[oh1])
        yield
        kb.stt(wt_all[:, ti, :], oh2[:], sm[:, 11:12], oh1[:], ALU.mult, ALU.add, R=[oh2, sm, oh1], W=[wt_all])

    NSL = 3
    slots = [WS(i) for i in range(NSL)]
    active = []
    nxt_ti = 0
    tick = 0
    while active or nxt_ti < NTO:
        if nxt_ti < NTO and len(active) < NSL and tick % 6 == 0:
            active.append(tile_gen(nxt_ti, slots[nxt_ti % NSL]))
            nxt_ti += 1
        tick += 1
        still = []
        for g_ in active:
            try:
                next(g_)
                still.append(g_)
            except StopIteration:
                pass
        active = still
    if DEBUG:
        kb.dma(kb.st, A["wtd"][:, :], wt_all[:].rearrange("p a b -> p (a b)"), R=[wt_all])


def moe_phase(kb, ph, nc, A, x1s, x1s_b, h2Ts, h2Ts_b, wt_all, gt2rep, y):
    NSB = 4
    TPS = NTO // NSB
    h2 = ph.sb("mh2", [128, 8, TPS * 128], BF16)
    acc = ph.sb("macc", [128, TPS, D], F32)
    st13 = [ph.sb(f"mst13{i}", [128, 8, 256], F32) for i in range(2)]
    st2 = [ph.sb(f"mst2{i}", [128, 2, D], F32) for i in range(2)]
    w1b = [ph.sb(f"mw1b{i}", [128, 8, 512], BF16) for i in range(2)]
    w3b = [ph.sb(f"mw3b{i}", [128, 8, 512], BF16) for i in range(2)]
    w2b = [ph.sb(f"mw2b{i}", [128, 4, D], BF16) for i in range(2)]
    sA = [ph.sb(f"msA{i}", [128, 512], F32) for i in range(3)]
    hid = [ph.sb(f"mhid{i}", [128, 512], BF16) for i in range(3)]
    hidT = [ph.sb(f"mhidT{i}", [128, 4, 128], BF16) for i in range(3)]
    sic = [0]
    pA = [ph.ps(f"mpA{i}", [128, 512], F32) for i in range(2)]
    pB = [ph.ps(f"mpB{i}", [128, 512], F32) for i in range(2)]
    pTr = [ph.ps(f"mpTr{i}", [128, 512], BF16) for i in range(2)]
    pO = [ph.ps(f"mpO{i}", [128, 512], F32) for i in range(2)]
    identb = ph.sb("midb", [128, 128], BF16)
    idf = ph.sb("midf", [128, 128], F32)
    kb.dma(kb.ld, idf[:], A["ident"][:, :], W=[idf])
    kb.cp(kb.dve, identb[:], idf[:], R=[idf], W=[identb])
    g2 = ph.sb("ln2g", [128, D], F32)
    b2 = ph.sb("ln2b", [128, D], F32)
    kb.dma(kb.ld, g2[:], A["ln2g"][:, :], W=[g2])
    kb.dma(kb.ld, b2[:], A["ln2b"][:, :], W=[b2])
    xt = [ph.sb(f"mxt{i}", [128, D], F32) for i in range(2)]
    yo = [ph.sb(f"myo{i}", [128, D], F32) for i in range(2)]
    lns = LNS(ph, "m")
    h2v = h2Ts.rearrange("(k p) t -> p k t", p=128)
    x1v = x1s.rearrange("(n p) d -> n p d", p=128)
    yv = y.rearrange("(n p) d -> n p d", p=128)
    si = 0
    it = 0
    for sbk in range(NSB):
        for k in range(8):
            kb.dma(kb.ld, h2[:, k, :], h2v[:, k, sbk * TPS * 128:(sbk + 1) * TPS * 128], R=[h2Ts_b], W=[h2])
        def load_pair(ep):
            wa, wc, wd = w1b[ep % 2], w3b[ep % 2], w2b[ep % 2]
            for j in range(2):
                e = ep * 2 + j
                for (src_, dst) in ((A["w1"], wa), (A["w3"], wc)):
                    s_ = st13[sic[0] % 2]
                    sic[0] += 1
                    kb.dma(kb.ld, s_[:], src_[e].rearrange("(k p) n -> p k n", p=128), W=[s_])
                    kb.cp(kb.pool if src_ is A["w1"] else kb.act, dst[:, :, j * 256:(j + 1) * 256], s_[:], R=[s_], W=[dst])
                s2 = st2[e % 2]
                kb.dma(kb.ld, s2[:], A["w2"][e].rearrange("(c p) n -> p c n", p=128), W=[s2])
                kb.cp(kb.pool if j == 0 else kb.act, wd[:, j * 2:(j + 1) * 2, :], s2[:], R=[s2], W=[wd])

        items = [(ep, tl) for ep in range(16) for tl in range(TPS)]

        def step1(i):
            ep, tl = items[i]
            ti = sbk * TPS + tl
            wa, wc = w1b[ep % 2], w3b[ep % 2]
            a_, b_ = pA[i % 2], pB[i % 2]
            sA_, hid_ = sA[i % 3], hid[i % 3]
            hs = lambda k: h2[:, k, tl * 128:(tl + 1) * 128]
            kb.mm(a_[:, :], [(hs(k), wa[:, k, :]) for k in range(8)], R=[h2, wa], W=[a_])
            kb.mm(b_[:, :], [(hs(k), wc[:, k, :]) for k in range(8)], R=[h2, wc], W=[b_])
            kb.actf(sA_[:], a_[:, :], AF.Silu, R=[a_], W=[sA_])
            for j in range(2):
                e = ep * 2 + j
                kb.stt(hid_[:, j * 256:(j + 1) * 256], sA_[:, j * 256:(j + 1) * 256], wt_all[:, ti, e:e + 1],
                       b_[:, j * 256:(j + 1) * 256], ALU.mult, ALU.mult, R=[sA_, wt_all, b_], W=[hid_])

        def step2(i):
            hid_, hT_, pt_ = hid[i % 3], hidT[i % 3], pTr[i % 2]
            for c in range(4):
                kb.tr(pt_[:, c * 128:(c + 1) * 128], hid_[:, c * 128:(c + 1) * 128], identb[:], R=[hid_, identb], W=[pt_])
            kb.cp(kb.act, hT_[:].rearrange("p c t -> p (c t)"), pt_[:, :], R=[pt_], W=[hT_])

        def step3(i):
            ep, tl = items[i]
            wd = w2b[ep % 2]
            hT_ = hidT[i % 3]
            for nb in range(2):
                o_ = pO[nb]
                kb.mm(o_[:, :], [(hT_[:, c, :], wd[:, c, nb * 512:(nb + 1) * 512]) for c in range(4)], R=[hT_, wd], W=[o_])
                dst = acc[:, tl, nb * 512:(nb + 1) * 512]
                if ep == 0:
                    kb.cp(kb.dve, dst, o_[:, :], R=[o_], W=[acc])
                else:
                    kb.tt(kb.dve, dst, dst, o_[:, :], ALU.add, R=[acc, o_], W=[acc])

        load_pair(0)
        for i in range(len(items) + 2):
            if i < len(items):
                ep, tl = items[i]
                if tl == 2 and ep + 1 < 16:
                    load_pair(ep + 1)
                step1(i)
            if 0 <= i - 1 < len(items):
                step2(i - 1)
            if 0 <= i - 2 < len(items):
                step3(i - 2)
        for tl in range(TPS):
            ti = sbk * TPS + tl
            xx = xt[ti % 2]
            yy = yo[ti % 2]
            kb.dma(kb.ld, xx[:], x1v[ti, :, :], R=[x1s_b], W=[xx])
            kb.tt(kb.dve, acc[:, tl, :], acc[:, tl, :], gt2rep[:], ALU.mult, R=[acc, gt2rep], W=[acc])
            kb.stt(yy[:], xx[:], ALPHA, acc[:, tl, :], ALU.mult, ALU.add, R=[xx, acc], W=[yy])
            stt_, mv, rstd, nmr = lns.next()
            ln_stats(kb, ph, yy, stt_, mv, rstd, nmr, "f")
            kb.actf(yy[:], yy[:], AF.Identity, R=[yy, rstd, nmr], W=[yy], scale=rstd[:, 0:1], bias=nmr[:, 0:1])
            kb.tt(kb.pool, yy[:], yy[:], g2[:], ALU.mult, R=[yy, g2], W=[yy])
            kb.tt(kb.pool, yy[:], yy[:], b2[:], ALU.add, R=[yy, b2], W=[yy])
            kb.dma(kb.st, yv[ti, :, :], yy[:], R=[yy])


def _interleave(gens):
    gens = list(gens)
    while gens:
        nxt_ = []
        for g in gens:
            try:
                next(g)
                nxt_.append(g)
            except StopIteration:
                pass
        gens = nxt_
        yield


def gdn_phase(kb, ph, nc, A, ident, identb, projT, projT_b, abtm, abtm_b, catT, catT_b):
    NM = 6
    mk = ph.sb("gmk", [128, 2 * NM, 128], BF16)
    cu32 = ph.sb("gcu32", [128, 2, 128], F32)
    with Phase(kb) as tph:
        mk32 = tph.sb("gmk32", [128, 2 * NM * 128], F32)
        kb.dma(kb.ld, mk32[:], A["masks"][:, :], W=[mk32])
        kb.cp(kb.pool, mk[:].rearrange("p m j -> p (m j)"), mk32[:], R=[mk32], W=[mk])
        for d_ in range(2):
            kb.cp(kb.dve, cu32[:, d_, :], mk32[:, (d_ * NM + 1) * 128:(d_ * NM + 2) * 128], R=[mk32], W=[cu32])
    mk32 = cu32
    MK = lambda d, m: mk[:, d * NM + m, :].unsqueeze(1).broadcast_to([128, 4, 128])
    CU32 = lambda d: cu32[:, d, :]
    id4 = identb
    ID4 = identb[:].unsqueeze(1).broadcast_to([128, 4, 128])
    V3 = lambda t: t[:].rearrange("p (h j) -> p h j", h=4)
    B3 = lambda ap4: ap4.unsqueeze(2).broadcast_to([128, 4, 128])
    ones32 = ph.sb("gones32", [128, 128], F32)
    onesb = ph.sb("gonesb", [128, 128], BF16)
    kb.op(kb.dve, lambda h: h.memset(ones32[:], 1.0), [], [ones32])
    kb.op(kb.dve, lambda h: h.memset(onesb[:], 1.0), [], [onesb])
    cw = ph.sb("gcw", [128, 60], F32)
    kb.dma(kb.ld, cw[:], A["convw"][:, :], W=[cw])
    dg = ph.sb("gdg", [128, 60, 128], BF16)
    for i in range(60):
        kb.ts(kb.dve, dg[:, i, :], identb[:], cw[:, i:i + 1], None, ALU.mult, R=[identb, cw], W=[dg])
    dtb = ph.sb("gdtb", [128, 8], F32)
    nA = ph.sb("gnA", [128, 8], F32)
    gng = ph.sb("ggng", [128, 1], F32)
    kb.dma(kb.ld, dtb[:], A["dtb"][:, :], W=[dtb])
    kb.dma(kb.ld, nA[:], A["alog"][:, :], W=[nA])
    kb.dma(kb.ld, gng[:], A["gng"][:, :], W=[gng])
    kb.actf(nA[:], nA[:], AF.Exp, R=[nA], W=[nA])
    kb.ts(kb.dve, nA[:], nA[:], -1.0, None, ALU.mult, R=[nA], W=[nA])
    one1 = const_col(kb, ph, "gone1", 1.0)
    epsk = const_col(kb, ph, "gepsk", RMS_EPS)
    epsq = const_col(kb, ph, "gepsq", RMS_EPS * 128.0)
    abt = ph.sb("gabt", [128, NT, 16], F32)
    kb.dma(kb.ld, abt[:].rearrange("p n c -> p (n c)"), abtm[:, :], W=[abt])
    ofd = A["ofd"]
    of_b = [Buf() for _ in range(NTO)]
    pp = [ph.ps(f"gpp{i}", [128, 512], F32) for i in range(6)]
    pb16 = [ph.ps(f"gpb{i}", [128, 512], BF16) for i in range(2)]
    cnt = {"p": 0, "b": 0}

    def nxt():
        cnt["p"] += 1
        return pp[cnt["p"] % 6]

    def nxtb():
        cnt["b"] += 1
        return pb16[cnt["b"] % 2]

    H = lambda t, h: t[:, h * 128:(h + 1) * 128]
    uid = [0]

    def W512(name, dt):
        uid[0] += 1
        return ph.sb(f"{name}{uid[0]}", [128, 512], dt)

    def sm(name, w):
        uid[0] += 1
        return ph.sb(f"{name}{uid[0]}", [128, w], F32)

    def mm4(lt, rt, R):
        p_ = nxt()
        for h in range(4):
            kb.mm(H(p_, h), [(H(lt, h), H(rt, h))], R=R, W=[p_])
        return p_

    class BT:
        def __init__(s):
            s.xin = [ph.sb(f"gxin{uid[0]}_{i}", [128, 516], BF16) for i in range(2)]
            uid[0] += 1
            s.kcT = W512("gkcT", BF16)
            s.sq, s.rs = W512("gsq", BF16), W512("grs", F32)
            s.li = 0

    class BS:
        def __init__(s, bt):
            s.bt = bt
            s.khT = [W512("gkhT", BF16) for _ in range(4)]
            s.qhT = [W512("gqhT", BF16) for _ in range(4)]
            s.vT = [W512("gvT", BF16) for _ in range(4)]

    class PI:
        def __init__(s):
            s.t8 = sm("gt8", 8)
            s.gc4, s.gl4, s.kd4, s.bg4 = [sm("gsm", 4) for _ in range(4)]
            s.gbc = [ph.sb(f"ggbc{uid[0]}_{i}", [128, 128], F32) for i in range(2)]
            uid[0] += 1
            s.dm, s.dmT = W512("gdm", F32), W512("gdmT", F32)
            s.dec, s.decT = W512("gdec", BF16), W512("gdecT", BF16)
            s.M4, s.MTs = W512("gM4", BF16), W512("gMTs", BF16)
            s.A_ = [W512("gA", BF16) for _ in range(2)]
            s.N_ = [W512("gN", BF16) for _ in range(2)]
            s.P_, s.Q_ = W512("gP", BF16), W512("gQ", BF16)
            s.Ml = [W512("gMl", BF16) for _ in range(3)]
            s.Nl = [W512("gNl", BF16) for _ in range(2)]
            s.X_, s.Y_ = s.A_[0], s.A_[1]
            s.kbg, s.bv = s.N_[0], s.N_[1]

    class CS:
        def __init__(s):
            s.g8, s.b8 = sm("gg8", 8), sm("gb8", 8)
            s.egc, s.dl4 = sm("gegc", 4), sm("gdl4", 4)
            s.kdec = W512("gkdec", BF16)
            s.wT4, s.u4, s.qkT4 = W512("gwT4", BF16), W512("gu4", F32), W512("gqkT4", BF16)

    class SS:
        def __init__(s, d):
            s.S32, s.S16 = W512("gS32", F32), W512("gS16", BF16)
            s.vn, s.oa4 = W512("gvn", BF16), W512("goa4", F32)
            if d == 0:
                s.ofs = [W512("gofs", BF16) for _ in range(2)]
            if d == 1:
                s.ofl = W512("gofl", BF16)
                s.o4 = W512("go4", F32)
                s.on4, s.zt, s.sz, s.gout, s.junk = [W512("gfin", BF16) for _ in range(5)]
                s.ss4 = sm("gss4", 4)

    def conv_group(bs, grp, row0, tb, dst):
        xi = bs.bt.xin[bs.bt.li % 2]
        bs.bt.li += 1
        lo, hi = tb * 512 - 2, tb * 512 + 514
        clo, chi = max(lo, 0), min(hi, S)
        if clo != lo or chi != hi:
            kb.op(kb.pool, lambda h: h.memset(xi[:], 0.0), [], [xi])
        kb.dma(kb.ld, xi[:, clo - lo:chi - lo], projT[row0:row0 + 128, clo:chi], W=[xi])
        p_ = nxt()
        kb.mm(p_[:, :], [(dg[:, grp * 5 + j, :], xi[:, j:j + 512]) for j in range(5)], R=[dg, xi], W=[p_])
        kb.actf(dst[:], p_[:, :], AF.Silu, R=[p_], W=[dst])

    def l2n(bs, src, dst, scale, epsc):
        kb.actf(bs.bt.sq[:], src[:], AF.Square, R=[src], W=[bs.bt.sq])
        p_ = nxt()
        kb.mm(p_[:, :], [(onesb[:], bs.bt.sq[:])], R=[onesb, bs.bt.sq], W=[p_])
        kb.actf(bs.bt.rs[:], p_[:, :], AF.Sqrt, R=[p_, epsc], W=[bs.bt.rs], scale=scale, bias=epsc[:, 0:1])
        kb.op(kb.dve, lambda hh: hh.reciprocal(out=bs.bt.rs[:], in_=bs.bt.rs[:]), [bs.bt.rs], [bs.bt.rs])
        kb.tt(kb.dve, dst[:], src[:], bs.bt.rs[:], ALU.mult, R=[src, bs.bt.rs], W=[dst])

    def blockprep(bs, tb):
        own = tb < OWN // 512
        for h in range(4):
            conv_group(bs, h, h * 128, tb, bs.bt.kcT)
            l2n(bs, bs.bt.kcT, bs.khT[h], 1.0, epsk)
            yield
            conv_group(bs, 4 + h, 512 + h * 128, tb, bs.vT[h])
            yield
            if own:
                conv_group(bs, 8 + h, 1536 + h * 128, tb, bs.bt.kcT)
                l2n(bs, bs.bt.kcT, bs.qhT[h], 128.0, epsq)
                yield

    def chunkprep(s, bs, n, c, d, own):
        cs = slice(c * 128, (c + 1) * 128)
        last = 127 if d == 0 else 0
        kb.tt(kb.dve, s.t8[:], abt[:, n, 0:8], dtb[:], ALU.add, R=[abt, dtb], W=[s.t8])
        kb.actf(s.t8[:], s.t8[:], AF.Exp, R=[s.t8], W=[s.t8])
        kb.actf(s.t8[:], s.t8[:], AF.Ln, R=[s.t8, one1], W=[s.t8], bias=one1[:, 0:1])
        kb.tt(kb.dve, s.g8[:], s.t8[:], nA[:], ALU.mult, R=[s.t8, nA], W=[s.g8])
        kb.actf(s.b8[:], abt[:, n, 8:16], AF.Sigmoid, R=[abt], W=[s.b8])
        yield
        gd = s.g8[:, d * 4:(d + 1) * 4]
        bd = s.b8[:, d * 4:(d + 1) * 4]
        pc = nxt()
        kb.mm(pc[:, 0:4], [(CU32(d), gd)], R=[mk32, s.g8], W=[pc])
        kb.cp(kb.dve, s.gc4[:], pc[:, 0:4], R=[pc], W=[s.gc4])
        pG = nxt()
        for h in range(4):
            gb_ = s.gbc[h % 2]
            kb.ts(kb.pool, gb_[:], ones32[:], s.g8[:, d * 4 + h:d * 4 + h + 1], None, ALU.mult, R=[ones32, s.g8], W=[gb_])
            kb.mm(H(pG, h), [(gb_[:], CU32(d))], R=[gb_, mk32], W=[pG])
        G3 = pG[:, :].rearrange("p (h j) -> p h j", h=4)
        kb.tt(kb.dve, V3(s.dmT), G3, B3(s.gc4[:]), ALU.subtract, R=[pG, s.gc4], W=[s.dmT])
        kb.cp(kb.dve, s.gl4[:], G3[:, :, last], R=[pG], W=[s.gl4])
        kb.actf(s.dm[:], s.dmT[:], AF.Relu, R=[s.dmT], W=[s.dm])
        kb.actf(s.dmT[:], s.dmT[:], AF.Relu, R=[s.dmT], W=[s.dmT], scale=-1.0)
        yield
        kb.actf(s.dec[:], s.dm[:], AF.Exp, R=[s.dm], W=[s.dec], scale=-1.0)
        kb.actf(s.decT[:], s.dmT[:], AF.Exp, R=[s.dmT], W=[s.decT], scale=-1.0)
        kb.tt(kb.pool, V3(s.dec), V3(s.dec), MK(d, 0), ALU.mult, R=[s.dec, mk], W=[s.dec])
        kb.tt(kb.pool, V3(s.decT), V3(s.decT), MK(d, 1), ALU.mult, R=[s.decT, mk], W=[s.decT])
        kb.actf(s.egc[:], s.gc4[:], AF.Exp, R=[s.gc4], W=[s.egc])
        kb.actf(s.dl4[:], s.gl4[:], AF.Exp, R=[s.gl4], W=[s.dl4])
        kb.tt(kb.dve, s.kd4[:], s.gl4[:], s.gc4[:], ALU.subtract, R=[s.gl4, s.gc4], W=[s.kd4])
        kb.actf(s.kd4[:], s.kd4[:], AF.Exp, R=[s.kd4], W=[s.kd4])
        kb.tt(kb.dve, s.bg4[:], s.egc[:], bd, ALU.mult, R=[s.egc, s.b8], W=[s.bg4])
        yield
        pK = nxt()
        for h in range(4):
            kb.mm(H(pK, h), [(bs.khT[h][:, cs], bs.khT[h][:, cs])], R=[bs.khT[h]], W=[pK])
        kb.tt(kb.dve, s.dm[:], pK[:, :], s.dec[:], ALU.mult, R=[pK, s.dec], W=[s.dm])
        kb.tt(kb.dve, V3(s.M4), V3(s.dm), B3(bd), ALU.mult, R=[s.dm, s.b8], W=[s.M4])
        yield
        pMT = nxtb()
        for h in range(4):
            kb.tr(H(pMT, h), H(s.M4, h), identb[:], R=[s.M4, identb], W=[pMT])
        kb.cp(kb.act, s.MTs[:], pMT[:, :], R=[pMT], W=[s.MTs])
        kb.tt(kb.pool, V3(s.A_[0]), V3(s.M4), MK(d, 2), ALU.mult, R=[s.M4, mk], W=[s.A_[0]])
        for l in range(3):
            kb.tt(kb.pool, V3(s.Ml[l]), V3(s.M4), MK(d, 3 + l), ALU.mult, R=[s.M4, mk], W=[s.Ml[l]])
        yield
        kb.tt(kb.pool, V3(s.N_[0]), V3(s.MTs), MK(1 - d, 2), ALU.mult, R=[s.MTs, mk], W=[s.N_[0]])
        for l in range(2):
            kb.tt(kb.pool, V3(s.Nl[l]), V3(s.MTs), MK(1 - d, 3 + l), ALU.mult, R=[s.MTs, mk], W=[s.Nl[l]])
        kb.tt(kb.dve, V3(s.P_), V3(s.A_[0]), ID4, ALU.add, R=[s.A_[0], id4], W=[s.P_])
        kb.tt(kb.dve, V3(s.Q_), V3(s.N_[0]), ID4, ALU.add, R=[s.N_[0], id4], W=[s.Q_])
        yield
        A_, N_, P_, Q_, Ml, Nl, X_, Y_ = s.A_, s.N_, s.P_, s.Q_, s.Ml, s.Nl, s.X_, s.Y_
        ca, cn = 0, 0
        for k in range(1, 4):
            pa_ = mm4(N_[cn], A_[ca], [N_[cn], A_[ca]])
            if k < 3:
                pn_ = mm4(A_[ca], N_[cn], [N_[cn], A_[ca]])
            kb.cp(kb.act, A_[1 - ca][:], pa_[:, :], R=[pa_], W=[A_[1 - ca]])
            if k < 3:
                kb.cp(kb.act, N_[1 - cn][:], pn_[:, :], R=[pn_], W=[N_[1 - cn]])
                cn = 1 - cn
            ca = 1 - ca
            yield
            pp_ = mm4(Q_, A_[ca], [Q_, A_[ca]])
            pq_ = mm4(A_[ca], Q_, [Q_, A_[ca]])
            kb.tt(kb.dve, P_[:], P_[:], pp_[:, :], ALU.add, R=[P_, pp_], W=[P_])
            kb.tt(kb.dve, Q_[:], Q_[:], pq_[:, :], ALU.add, R=[Q_, pq_], W=[Q_])
            yield
        for l in range(3):
            if l < 2:
                py = mm4(Nl[l], P_, [Nl[l], P_])
                kb.cp(kb.act, Y_[:], py[:, :], R=[py], W=[Y_])
            px = mm4(Ml[l], Q_, [Ml[l], Q_])
            kb.cp(kb.act, X_[:], px[:, :], R=[px], W=[X_])
            yield
            if l < 2:
                pt_ = mm4(Q_, Y_, [Q_, Y_])
            pu_ = mm4(P_, X_, [P_, X_])
            if l < 2:
                kb.tt(kb.dve, P_[:], P_[:], pt_[:, :], ALU.subtract, R=[P_, pt_], W=[P_])
            kb.tt(kb.dve, Q_[:], Q_[:], pu_[:, :], ALU.subtract, R=[Q_, pu_], W=[Q_])
            yield
        pkT = nxtb()
        for h in range(4):
            kb.tr(H(pkT, h), bs.khT[h][:, cs], identb[:], R=[bs.khT[h], identb], W=[pkT])
        pkT3 = pkT[:, :].rearrange("p (h j) -> p h j", h=4)
        kb.tt(kb.dve, V3(s.kbg), pkT3, B3(s.bg4[:]), ALU.mult, R=[pkT, s.bg4], W=[s.kbg])
        kb.tt(kb.dve, V3(s.kdec), pkT3, B3(s.kd4[:]), ALU.mult, R=[pkT, s.kd4], W=[s.kdec])
        yield
        pvT = nxtb()
        for h in range(4):
            kb.tr(H(pvT, h), bs.vT[h][:, cs], identb[:], R=[bs.vT[h], identb], W=[pvT])
        kb.tt(kb.dve, V3(s.bv), pvT[:, :].rearrange("p (h j) -> p h j", h=4), B3(bd), ALU.mult, R=[pvT, s.b8], W=[s.bv])
        yield
        pw = mm4(s.kbg, Q_, [s.kbg, Q_])
        kb.cp(kb.act, s.wT4[:], pw[:, :], R=[pw], W=[s.wT4])
        pu = mm4(Q_, s.bv, [Q_, s.bv])
        kb.cp(kb.act, s.u4[:], pu[:, :], R=[pu], W=[s.u4])
        yield
        if own:
            pqk = nxt()
            for h in range(4):
                kb.mm(H(pqk, h), [(bs.khT[h][:, cs], bs.qhT[h][:, cs])], R=[bs.khT[h], bs.qhT[h]], W=[pqk])
            kb.tt(kb.dve, s.qkT4[:], pqk[:, :], s.decT[:], ALU.mult, R=[pqk, s.decT], W=[s.qkT4])
            yield

    def scan(s, bs, z, n, c, d, own):
        cs = slice(c * 128, (c + 1) * 128)
        pv_ = mm4(s.wT4, z.S16, [s.wT4, z.S16])
        kb.tt(kb.dve, z.vn[:], s.u4[:], pv_[:, :], ALU.subtract, R=[s.u4, pv_], W=[z.vn])
        yield
        ps_ = mm4(s.kdec, z.vn, [s.kdec, z.vn])
        if own:
            pa2 = nxt()
            for h in range(4):
                kb.mm(H(pa2, h), [(bs.qhT[h][:, cs], H(z.S16, h))], R=[bs.qhT[h], z.S16], W=[pa2])
            pb2 = mm4(s.qkT4, z.vn, [s.qkT4, z.vn])
        kb.tt(kb.dve, V3(z.S32), V3(z.S32), B3(s.dl4[:]), ALU.mult, R=[z.S32, s.dl4], W=[z.S32])
        kb.tt(kb.dve, z.S32[:], z.S32[:], ps_[:, :], ALU.add, R=[z.S32, ps_], W=[z.S32])
        if own:
            kb.tt(kb.dve, V3(z.oa4), pa2[:, :].rearrange("p (h j) -> p h j", h=4), B3(s.egc[:]), ALU.mult, R=[pa2, s.egc], W=[z.oa4])
            if d == 0:
                ofs_ = z.ofs[n % 2]
                kb.tt(kb.dve, ofs_[:], z.oa4[:], pb2[:, :], ALU.add, R=[z.oa4, pb2], W=[ofs_])
                kb.dma(kb.st, ofd[n, :, :], ofs_[:], R=[ofs_], W=[of_b[n]])
            else:
                kb.tt(kb.dve, z.o4[:], z.oa4[:], pb2[:, :], ALU.add, R=[z.oa4, pb2], W=[z.o4])
        kb.cp(kb.act, z.S16[:], z.S32[:], R=[z.S32], W=[z.S16])
        yield
        if own and d == 1:
            kb.dma(kb.ld, z.ofl[:], ofd[n, :, :], R=[of_b[n]], W=[z.ofl])
            kb.tt(kb.pool, z.o4[:], z.o4[:], z.ofl[:], ALU.add, R=[z.o4, z.ofl], W=[z.o4])
            kb.tt(kb.pool, z.oa4[:], z.o4[:], z.o4[:], ALU.mult, R=[z.o4], W=[z.oa4])
            kb.op(kb.dve, lambda hh: hh.tensor_reduce(out=z.ss4[:], in_=V3(z.oa4), axis=AX.X, op=ALU.add), [z.oa4], [z.ss4])
            kb.ts(kb.dve, z.ss4[:], z.ss4[:], 1.0 / 128.0, RMS_EPS, ALU.mult, ALU.add, R=[z.ss4], W=[z.ss4])
            kb.actf(z.ss4[:], z.ss4[:], AF.Sqrt, R=[z.ss4], W=[z.ss4])
            kb.op(kb.dve, lambda hh: hh.reciprocal(out=z.ss4[:], in_=z.ss4[:]), [z.ss4], [z.ss4])
            yield
            kb.tt(kb.pool, V3(z.on4), V3(z.o4), B3(z.ss4[:]), ALU.mult, R=[z.o4, z.ss4], W=[z.on4])
            for h in range(4):
                kb.dma(kb.ld, H(z.zt, h), projT[2048 + h * 128:2048 + (h + 1) * 128, n * 128:(n + 1) * 128], W=[z.zt])
            kb.actf(z.sz[:], z.zt[:], AF.Silu, R=[z.zt], W=[z.sz])
            yield
            pot = nxtb()
            for h in range(4):
                kb.tr(H(pot, h), H(z.on4, h), identb[:], R=[z.on4, identb], W=[pot])
            kb.stt(z.gout[:], pot[:, :], gng[:, 0:1], z.sz[:], ALU.mult, ALU.mult, R=[pot, gng, z.sz], W=[z.gout])
            kb.dma(kb.st, catT[0:512, n * 128:(n + 1) * 128].rearrange("(h p) t -> p h t", p=128),
                   z.gout[:].rearrange("p (h t) -> p h t", h=4), R=[z.gout])
        yield

    class View:
        def __init__(v, pi, cs):
            v.__dict__.update(pi.__dict__)
            v.__dict__.update(cs.__dict__)

    def chain(d):
        z = SS(d)
        bt = BT()
        bss = [BS(bt), BS(bt)]
        NP = 2 if d == 1 else 1
        NCS = NP + 1
        pis = [PI() for _ in range(NP)]
        css = [CS() for _ in range(NCS)]
        kb.op(kb.dve, lambda h: h.memset(z.S32[:], 0.0), [], [z.S32])
        kb.op(kb.dve, lambda h: h.memset(z.S16[:], 0.0), [], [z.S16])
        blocks = list(range(OWN // 512)) if d == 0 else list(range(S // 512 - 1, -1, -1))
        items = []
        for bi, tb in enumerate(blocks):
            for c in (range(4) if d == 0 else range(3, -1, -1)):
                items.append((bi, tb, c))
        N = len(items)
        blk_ready = set()
        blk_gen, cur_blk, blk_next = None, None, 0
        prep_state = [None] * NP
        prep_next, prep_done = 0, set()
        scan_i, scan_gen, scans_emitted = 0, None, 0
        while scans_emitted < N:
            if blk_gen is None and blk_next < len(blocks):
                if blk_next < 2 or scans_emitted >= 4 * (blk_next - 1):
                    cur_blk = blk_next
                    blk_gen = blockprep(bss[blk_next % 2], blocks[blk_next])
                    blk_next += 1
            for p in range(NP):
                if prep_state[p] is None and prep_next < N:
                    i = prep_next
                    bi, tb, c = items[i]
                    if bi in blk_ready and (i < NCS or scans_emitted >= i - NCS + 1):
                        prep_state[p] = (i, chunkprep(View(pis[p], css[i % NCS]), bss[bi % 2], tb * 4 + c, c, d, tb < OWN // 512))
                        prep_next += 1
            if scan_gen is None and scan_i < N and scan_i in prep_done:
                bi, tb, c = items[scan_i]
                if not (d == 1 and tb < OWN // 512 and not fwd_done[0]):
                    scan_gen = scan(css[scan_i % NCS], bss[bi % 2], z, tb * 4 + c, c, d, tb < OWN // 512)
            if blk_gen is not None:
                try:
                    next(blk_gen)
                except StopIteration:
                    blk_ready.add(cur_blk)
                    blk_gen = None
            for p in range(NP):
                if prep_state[p] is not None:
                    try:
                        next(prep_state[p][1])
                    except StopIteration:
                        prep_done.add(prep_state[p][0])
                        prep_state[p] = None
            if scan_gen is not None:
                try:
                    next(scan_gen)
                except StopIteration:
                    scans_emitted += 1
                    scan_i += 1
                    scan_gen = None
            yield
        if d == 0:
            fwd_done[0] = True

    fwd_done = [False]
    for _ in _interleave([chain(0), chain(1)]):
        pass


def build_program():
    nc = bass.Bass("TRN2", target_bir_lowering=False)
    es = contextlib.ExitStack()
    dram = lambda n, shp, dt=F32, kind="ExternalInput": nc.dram_tensor(n, shp, dt, kind=kind).ap()
    x = dram("x", [S, D])
    cvec = dram("cvec", [128, 8])
    w_ada = dram("w_ada", [D, 6 * D])
    b_ada = dram("b_ada", [128, 48])
    w_in = dram("w_in", [D, 3088])
    ident_d = dram("ident", [128, 128])
    y = dram("y", [OWN, D], kind="ExternalOutput")
    projT = dram("projT", [3072, S], BF16, kind="Internal")
    abtm = dram("abtm", [128, NT * 16], F32, kind="Internal")
    modT_d = dram("modT_d", [128, 48], F32, kind="ExternalOutput" if DEBUG else "Internal")


    A = {}
    for n, shp in [("cosT", [128, S]), ("sinT", [128, S]), ("pm_d", [128, 128]), ("qg", [128, 1]), ("kg", [128, 1]),
                   ("ang", [128, 4]), ("w_out", [D, D]), ("ln1g", [128, D]), ("ln1b", [128, D]), ("ln2g", [128, D]),
                   ("ln2b", [128, D]), ("wr", [D, 36]), ("br", [128, 36]), ("w1", [32, D, 256]), ("w3", [32, D, 256]),
                   ("w2", [32, 256, D]), ("convw", [128, 60]), ("dtb", [128, 8]), ("alog", [128, 8]), ("gng", [128, 1]),
                   ("masks", [128, 12 * 128])]:
        A[n] = dram(n, shp)
    A["ident"] = ident_d
    dk = "ExternalOutput" if DEBUG else "Internal"
    catT = dram("catT", [D, OWN], BF16, kind=dk)
    x1s = dram("x1s", [OWN, D], F32, kind=dk)
    h2Ts = dram("h2Ts", [D, OWN], BF16, kind=dk)
    A["wtd"] = dram("wtd", [128, NTO * 32], F32, kind=dk)
    A["ofd"] = dram("ofd", [NTO, 128, 512], BF16, kind="Internal")
    catT_b, x1s_b, h2Ts_b = Buf(), Buf(), Buf()
    kb = KB(nc, es)
    glob = Phase(kb)
    glob.__enter__()
    ident = glob.sb("ident_s", [128, 128], F32)
    identb = glob.sb("identb", [128, 128], BF16)
    modT = glob.sb("modT", [128, 48], F32)
    sc1p = glob.sb("sc1p", [128, 8], F32)
    sc2p = glob.sb("sc2p", [128, 8], F32)
    kb.dma(kb.ld, ident[:], ident_d[:, :], W=[ident])
    kb.cp(kb.dve, identb[:], ident[:], R=[ident], W=[identb])
    projT_b = Buf()
    abtm_b = Buf()

    with Phase(kb) as ph:
        cs = ph.sb("cs", [128, 8], F32)
        bad = ph.sb("bad", [128, 48], F32)
        wa = [ph.sb(f"wa{i}", [128, 8, 512], F32) for i in range(2)]
        pm = ph.ps("pm", [128, 48], F32)
        kb.dma(kb.ld, cs[:], cvec[:, :], W=[cs])
        kb.dma(kb.ld, bad[:], b_ada[:, :], W=[bad])
        kb.actf(cs[:], cs[:], AF.Silu, R=[cs], W=[cs])
        wav = w_ada.rearrange("(k p) n -> p k n", p=128)
        for jb in range(12):
            w = wa[jb % 2]
            kb.dma(kb.ld, w[:], wav[:, :, jb * 512:(jb + 1) * 512], W=[w])
            for jj in range(4):
                j = jb * 4 + jj
                kb.mm(pm[:, j:j + 1], [(w[:, k, jj * 128:(jj + 1) * 128], cs[:, k:k + 1]) for k in range(8)],
                      R=[w, cs], W=[pm])
        kb.tt(kb.dve, modT[:], pm[:], bad[:], ALU.add, R=[pm, bad], W=[modT])
        kb.ts(kb.dve, sc1p[:], modT[:, 8:16], 1.0, None, ALU.add, R=[modT], W=[sc1p])
        kb.ts(kb.dve, sc2p[:], modT[:, 32:40], 1.0, None, ALU.add, R=[modT], W=[sc2p])
        if DEBUG:
            kb.dma(kb.st, modT_d[:, :], modT[:], R=[modT])

    if STAGE >= 1:
        with Phase(kb) as ph:
            wbf = ph.sb("wbf", [128, 8, 3088], BF16)
            wst = [ph.sb(f"wst{i}", [128, 3088], F32) for i in range(2)]
            wv = w_in.rearrange("(k p) n -> p k n", p=128)
            for k in range(8):
                w = wst[k % 2]
                kb.dma(kb.ld, w[:], wv[:, k, :], W=[w])
                kb.cp(kb.pool, wbf[:, k, :], w[:], R=[w], W=[wbf])
            xt = [ph.sb(f"xt{i}", [128, D], F32) for i in range(3)]
            xn = [ph.sb(f"xn{i}", [128, D], F32) for i in range(2)]
            hT = [ph.sb(f"hT{i}", [128, 8, 512], BF16) for i in range(2)]
            lns = LNS(ph, "b")
            ptr = [ph.ps(f"ptr{i}", [128, 512], F32) for i in range(2)]
            pmm = [ph.ps(f"pmm{i}", [128, 512], F32) for i in range(3)]
            pab = ph.ps("pab", [128, 16], F32)
            stg = [ph.sb(f"stg{i}", [128, 512], BF16) for i in range(4)]
            abs_ = ph.sb("abs", [128, NT * 16], F32)
            xv = x.rearrange("(n p) d -> n p d", p=128)
            it = 0
            ig = 0
            igc = [0]

            def ln_tile(tb, tt_):
                h_ = hT[tb % 2]
                ti = tb * 4 + tt_
                xx = xt[ti % 3]
                xo = xn[ti % 2]
                kb.dma(kb.ld, xx[:], xv[ti, :, :], W=[xx])
                stt_, mv, rstd, nmr = lns.next()
                ln_stats(kb, ph, xx, stt_, mv, rstd, nmr, "b")
                kb.actf(xo[:], xx[:], AF.Identity, R=[xx, rstd, nmr], W=[xo], scale=rstd[:, 0:1], bias=nmr[:, 0:1])
                for half in range(2):
                    p_ = ptr[half]
                    for f in range(4):
                        kb.tr(p_[:, f * 128:(f + 1) * 128], xo[:, (half * 4 + f) * 128:(half * 4 + f + 1) * 128], ident[:],
                              R=[xo, ident], W=[p_])
                    for f in range(4):
                        fc = half * 4 + f
                        kb.actf(h_[:, fc, tt_ * 128:(tt_ + 1) * 128], p_[:, f * 128:(f + 1) * 128], AF.Identity,
                                R=[p_, sc1p, modT], W=[h_], scale=sc1p[:, fc:fc + 1], bias=modT[:, fc:fc + 1])
                kb.mm(pab[:, :], [(h_[:, k, tt_ * 128:(tt_ + 1) * 128], wbf[:, k, 3072:3088]) for k in range(8)],
                      R=[h_, wbf], W=[pab])
                kb.cp(kb.dve, abs_[:, ti * 16:(ti + 1) * 16], pab[:, :], R=[pab], W=[abs_])

            def mm_group(tb, cg):
                h_ = hT[tb % 2]
                ig = igc[0]
                igc[0] += 1
                p_ = pmm[ig % 3]
                s_ = stg[ig % 4]
                kb.mm(p_[:, :], [(wbf[:, k, cg * 128:(cg + 1) * 128], h_[:, k, :]) for k in range(8)],
                      R=[wbf, h_], W=[p_])
                if ig % 2 == 0:
                    kb.cp(kb.dve, s_[:], p_[:, :], R=[p_], W=[s_])
                else:
                    kb.cp(kb.act, s_[:], p_[:, :], R=[p_], W=[s_])
                kb.dma(kb.st, projT[cg * 128:(cg + 1) * 128, tb * 512:(tb + 1) * 512], s_[:], R=[s_])

            NB = S // 512
            for tt_ in range(4):
                ln_tile(0, tt_)
            for tb in range(NB):
                ngrp = 24 if tb < OWN // 512 else (16 if tb == OWN // 512 else 12)
                per = ngrp // 4
                for j in range(4):
                    for cg in range(j * per, (j + 1) * per):
                        mm_group(tb, cg)
                    if tb + 1 < NB:
                        ln_tile(tb + 1, j)
            kb.dma(kb.st, abtm[:, :], abs_[:], R=[abs_])


    if STAGE >= 2:
        with Phase(kb) as ph:
            attention_phase(kb, ph, nc, A, ident, identb, projT, projT_b, catT, catT_b)
    if STAGE >= 3 and not SKIP_GDN:
        with Phase(kb) as ph:
            gdn_phase(kb, ph, nc, A, ident, identb, projT, projT_b, abtm, abtm_b, catT, catT_b)
    elif STAGE >= 3:
        with Phase(kb) as ph:
            zt = ph.sb("zt", [128, 4096], BF16)
            kb.op(kb.dve, lambda h: h.memset(zt[:], 0.0), [], [zt])
            for r in range(4):
                kb.dma(kb.st, catT[r * 128:(r + 1) * 128, :], zt[:], R=[zt])
    if STAGE >= 4:
        wt_all = glob.sb("wt_all", [128, NTO, 32], F32)
        gt2rep = glob.sb("gt2rep", [128, D], F32)
        with Phase(kb) as ph:
            wout_phase(kb, ph, nc, A, ident, identb, modT, sc2p, catT, catT_b, x, x1s, x1s_b, h2Ts, h2Ts_b, wt_all, gt2rep)
    if STAGE >= 5:
        with Phase(kb) as ph:
            moe_phase(kb, ph, nc, A, x1s, x1s_b, h2Ts, h2Ts_b, wt_all, gt2rep, y)
    if STAGE < 99:
        with Phase(kb) as ph:
            z = ph.sb("z", [128, D], F32)
            kb.op(kb.dve, lambda h: h.memset(z[:], 0.0), [], [z])
            kb.dma(kb.st, y[0:128, :], z[:], R=[z])
    kb.final_wait()
    glob.__exit__(None, None, None)
    es.close()
    return nc


def _rope_tables(pos_tok):
    inv = 10000.0 ** (-np.arange(0, 64, 2, dtype=np.float32) / 64.0)
    row = (pos_tok // 64).astype(np.float32)
    col = (pos_tok % 64).astype(np.float32)
    ang = np.zeros((128, pos_tok.shape[0]), np.float32)
    for d in range(128):
        p = row if d < 64 else col
        ang[d] = p * inv[d % 32]
    return np.cos(ang).astype(np.float32), np.sin(ang).astype(np.float32)


def _gdn_masks():
    i = np.arange(128)
    bd = lambda s: ((i[:, None] // s) == (i[None, :] // s)).astype(np.float32)
    sl = (i[:, None] > i[None, :]).astype(np.float32)
    out = []
    for d in range(2):
        s_ = sl if d == 0 else sl.T
        cu = (i[:, None] <= i[None, :]).astype(np.float32) if d == 0 else (i[:, None] >= i[None, :]).astype(np.float32)
        out += [s_, cu, -bd(16) * s_, (bd(32) - bd(16)) * s_, (bd(64) - bd(32)) * s_, (1.0 - bd(64)) * s_]
    return np.concatenate(out, axis=1).astype(np.float32)


def make_in_maps(inputs):
    maps = []
    ident = np.eye(128, dtype=np.float32)
    f = lambda k: np.asarray(inputs[k], np.float32)
    x = f("x")
    c = f("c")
    w_in = f("w_in")[0]
    gq, gk, gv, gz = w_in[:, 0:512], w_in[:, 512:1024], w_in[:, 1024:1536], w_in[:, 1536:2048]
    ab = w_in[:, 2048:2064]
    aq, ak, av = w_in[:, 2064:2576], w_in[:, 2576:2832], w_in[:, 2832:3088]
    pm = np.zeros((128, 128), np.float32)
    for d in range(128):
        if (d % 64) < 32:
            pm[d + 32, d] = -1.0
        else:
            pm[d - 32, d] = 1.0
    rep = lambda v: np.ascontiguousarray(np.broadcast_to(np.asarray(v, np.float32).reshape(1, -1), (128, np.asarray(v).size)))
    conv = f("conv_w")[0]
    a_log = f("a_log")[0]
    dt_bias = f("dt_bias")[0]
    masks = _gdn_masks()
    shared = {
        "w_ada": np.ascontiguousarray(f("w_ada")[0]),
        "b_ada": np.ascontiguousarray(f("b_ada")[0].reshape(48, 128).T),
        "ident": ident, "pm_d": pm,
        "qg": np.ascontiguousarray(f("q_norm_g")[0].reshape(128, 1)),
        "kg": np.ascontiguousarray(f("k_norm_g")[0].reshape(128, 1)),
        "ang": np.ascontiguousarray(f("attn_norm_g")[0].reshape(4, 128).T),
        "w_out": np.ascontiguousarray(f("w_out")[0]),
        "ln1g": rep(f("ln1_g")[0]), "ln1b": rep(f("ln1_b")[0]), "ln2g": rep(f("ln2_g")[0]), "ln2b": rep(f("ln2_b")[0]),
        "wr": np.ascontiguousarray(np.concatenate([f("w_group")[0], f("w_router")[0]], axis=1)),
        "br": rep(np.concatenate([f("b_group")[0], f("b_router")[0]])),
        "w1": np.ascontiguousarray(f("w1")[0]), "w3": np.ascontiguousarray(f("w3")[0]), "w2": np.ascontiguousarray(f("w2")[0]),
        "gng": np.ascontiguousarray(f("gdn_norm_g")[0].reshape(128, 1)),
        "masks": masks,
    }
    for core in range(8):
        b, half = core // 2, core % 2
        pos = np.arange(S) if half == 0 else np.arange(S)[::-1]
        xl = x[b] if half == 0 else x[b][::-1]
        a_f, a_b, b_f, b_b = ab[:, 0:4], ab[:, 4:8], ab[:, 8:12], ab[:, 12:16]
        if half == 0:
            abl = np.concatenate([a_f, a_b, b_f, b_b], axis=1)
            dirs = [0, 1]
            cw = conv
        else:
            abl = np.concatenate([a_b, a_f, b_b, b_f], axis=1)
            dirs = [1, 0]
            cw = conv[::-1]
        w_in_p = np.concatenate([gk, gv, ak, av, gq, gz, aq, abl], axis=1)
        cwp = np.concatenate([cw[:, 512:1024], cw[:, 1024:1536], cw[:, 0:512]], axis=1)
        convw = np.ascontiguousarray(cwp.T.reshape(12, 128, 5).transpose(1, 0, 2).reshape(128, 60))
        cosT, sinT = _rope_tables(pos)
        m = dict(shared)
        m.update({
            "x": np.ascontiguousarray(xl),
            "cvec": np.ascontiguousarray(c[b].reshape(8, 128).T),
            "w_in": np.ascontiguousarray(w_in_p),
            "cosT": cosT, "sinT": sinT,
            "convw": convw,
            "dtb": rep(np.concatenate([dt_bias[dirs[0]], dt_bias[dirs[1]]])),
            "alog": rep(np.concatenate([a_log[dirs[0]], a_log[dirs[1]]])),
        })
        maps.append(m)
    return maps


def kernel(**inputs):
    nc = build_program()
    maps = make_in_maps(inputs)
    res = run_bass_kernel_spmd(nc, maps, core_ids=list(range(8)))
    out = np.zeros((4, S, D), np.float32)
    for core in range(8):
        b, half = core // 2, core % 2
        yl = np.asarray(res.results[core]["y"], np.float32)
        if half == 0:
            out[b, :OWN] = yl
        else:
            out[b, OWN:] = yl[::-1]
    kernel.last = res
    return out
```
